# Optimizing a Trainium2 kernel written in Bass

```python
import jax
import jax.numpy as jnp
from jax import lax
import numpy as np


D_MODEL = 2048
BATCH = 4
SEQ = 2048
DEPTH = 1

CHUNK = 64
SPATIAL_BLOCK = 128
A_WIDTH = D_MODEL
A_GROUPS = 8
A_GROUP_DIM = A_WIDTH // A_GROUPS
GLA_HEADS = 4
GLA_DK = D_MODEL // 2
GLA_DV = D_MODEL
GLA_HEAD_K = GLA_DK // GLA_HEADS
GLA_HEAD_V = GLA_DV // GLA_HEADS
GLA_GATE_RANK = 16
GLA_GATE_TAU = 16.0
N_EXPERTS = 32
TOP_K = 4
D_FF = D_MODEL
SWIGLU_LIMIT = 7.0
SWIGLU_ALPHA = 1.702
EXPERT_BLOCK = 256
NORM_EPS = 1e-6
IN_SPLITS = (A_WIDTH, A_WIDTH, GLA_DK, GLA_DK, GLA_DV, GLA_DV, GLA_GATE_RANK, D_MODEL, D_MODEL)
IN_WIDTH = 2 * A_WIDTH + 2 * GLA_DK + 2 * GLA_DV + GLA_GATE_RANK + 2 * D_MODEL

kernel_name = 'hybrid_gmlp_gla_moe_adaln_block'


def _split_points():
    pts, acc = [], 0
    for w in IN_SPLITS[:-1]:
        acc += w
        pts.append(acc)
    return pts


def rms_norm(x, g):
    xf = x.astype(jnp.float32)
    y = xf * lax.rsqrt(jnp.mean(xf * xf, axis=-1, keepdims=True) + NORM_EPS)
    return (y * g.astype(jnp.float32)).astype(x.dtype)


def layer_norm(x, g, b):
    xf = x.astype(jnp.float32)
    mu = jnp.mean(xf, axis=-1, keepdims=True)
    xc = xf - mu
    var = jnp.mean(xc * xc, axis=-1, keepdims=True)
    return (xc * lax.rsqrt(var + NORM_EPS) * g.astype(jnp.float32) + b.astype(jnp.float32)).astype(x.dtype)


def spatial_gating(u, v, ln_g, ln_b, w_s, b_s):
    B, S, _ = v.shape
    nb = S // SPATIAL_BLOCK
    vn = layer_norm(v, ln_g, ln_b).reshape(B, nb, SPATIAL_BLOCK, A_GROUPS, A_GROUP_DIM)
    chunk_id = jnp.arange(SPATIAL_BLOCK) // CHUNK
    mask = chunk_id[None, :] <= chunk_id[:, None]
    w = jnp.where(mask[None], w_s, 0.0)
    mixed = jnp.einsum('gts,bnsgc->bntgc', w, vn) + b_s.T[None, None, :, :, None]
    return u * mixed.reshape(B, S, A_WIDTH)


def gated_linear_attention(q, k, v, log_a):
    B, S, H, dk = q.shape
    dv = v.shape[-1]
    n = S // CHUNK
    f32 = jnp.float32
    q = q.astype(f32).reshape(B, n, CHUNK, H, dk) * (dk ** -0.5)
    k = k.astype(f32).reshape(B, n, CHUNK, H, dk)
    v = v.astype(f32).reshape(B, n, CHUNK, H, dv)
    b = jnp.cumsum(log_a.astype(f32).reshape(B, n, CHUNK, H, dk), axis=2)
    b_last = b[:, :, -1:]
    q_dec = q * jnp.exp(b)
    k_intra = k * jnp.exp(-b)
    k_state = k * jnp.exp(b_last - b)
    causal = jnp.tril(jnp.ones((CHUNK, CHUNK), dtype=bool))
    att = jnp.einsum('bnthk,bnshk->bnhts', q_dec, k_intra)
    att = jnp.where(causal, att, 0.0)
    o_intra = jnp.einsum('bnhts,bnshv->bnthv', att, v)

    def step(state, inp):
        qc, kc, vc, dc = inp
        o = jnp.einsum('blhk,bhkv->blhv', qc, state)
        state = dc[..., None] * state + jnp.einsum('blhk,blhv->bhkv', kc, vc)
        return state, o

    xs = (jnp.moveaxis(q_dec, 1, 0), jnp.moveaxis(k_state, 1, 0), jnp.moveaxis(v, 1, 0),
          jnp.moveaxis(jnp.exp(b_last[:, :, 0]), 1, 0))
    s0 = jnp.zeros((B, H, dk, dv), f32)
    _, o_inter = lax.scan(step, s0, xs)
    o = o_intra + jnp.moveaxis(o_inter, 0, 1)
    return o.reshape(B, S, H, dv)


def routed_experts(h, router_w, router_b, w_gate, b_gate, w_up, b_up, w_down, b_down):
    B, S, D = h.shape
    T = B * S
    n_rows = T * TOP_K
    n_blocks = (n_rows + N_EXPERTS * (EXPERT_BLOCK - 1) + EXPERT_BLOCK - 1) // EXPERT_BLOCK
    hf = h.reshape(T, D)
    logits = (hf @ router_w + router_b).astype(jnp.float32)
    top_logit, top_idx = lax.top_k(logits, TOP_K)
    top_w = jax.nn.softmax(top_logit, axis=-1)
    flat_e = top_idx.reshape(n_rows)
    flat_tok = jnp.repeat(jnp.arange(T, dtype=jnp.int32), TOP_K)
    flat_w = top_w.reshape(n_rows)
    order = jnp.argsort(flat_e)
    sorted_e = flat_e[order]
    counts = jnp.bincount(flat_e, length=N_EXPERTS)
    starts = jnp.cumsum(counts) - counts
    padded = (counts + EXPERT_BLOCK - 1) // EXPERT_BLOCK * EXPERT_BLOCK
    padded_end = jnp.cumsum(padded)
    padded_start = padded_end - padded
    dest = padded_start[sorted_e] + jnp.arange(n_rows) - starts[sorted_e]
    row_tok = jnp.zeros((n_blocks * EXPERT_BLOCK,), jnp.int32).at[dest].set(flat_tok[order])
    row_w = jnp.zeros((n_blocks * EXPERT_BLOCK,), jnp.float32).at[dest].set(flat_w[order])
    block_e = jnp.minimum(jnp.searchsorted(padded_end, jnp.arange(n_blocks) * EXPERT_BLOCK, side='right'),
                          N_EXPERTS - 1)

    def step(acc, inp):
        e, tok, w = inp
        xb = hf[tok]
        g = jnp.minimum(xb @ w_gate[e] + b_gate[e], SWIGLU_LIMIT)
        u = jnp.clip(xb @ w_up[e] + b_up[e], -SWIGLU_LIMIT, SWIGLU_LIMIT)
        y = ((u + 1.0) * (g * jax.nn.sigmoid(SWIGLU_ALPHA * g))) @ w_down[e] + b_down[e]
        return acc.at[tok].add(y.astype(jnp.float32) * w[:, None]), None

    acc, _ = lax.scan(step, jnp.zeros((T, D), jnp.float32),
                      (block_e, row_tok.reshape(n_blocks, EXPERT_BLOCK), row_w.reshape(n_blocks, EXPERT_BLOCK)))
    return acc.reshape(B, S, D).astype(h.dtype)


def setup_inputs(seed: int = 0) -> dict:
    key = jax.random.key(seed)
    ks = jax.random.split(key, 26)
    f32 = jnp.float32
    L, D = DEPTH, D_MODEL

    def nrm(k, shape, scale):
        return jax.random.normal(k, shape, f32) * scale

    return {
        'x': nrm(ks[0], (BATCH, SEQ, D), 1.0),
        'c': nrm(ks[1], (BATCH, D), 1.0),
        'ada_w': nrm(ks[2], (L, D, 6 * D), 0.5 * D ** -0.5),
        'ada_b': nrm(ks[3], (L, 6 * D), 0.02),
        'norm1_g': 1.0 + nrm(ks[4], (L, D), 0.02),
        'w_in': nrm(ks[5], (L, D, IN_WIDTH), D ** -0.5),
        'gla_gate_w2': nrm(ks[6], (L, GLA_GATE_RANK, GLA_DK), GLA_GATE_RANK ** -0.5),
        'gla_gate_b': nrm(ks[7], (L, GLA_DK), 0.1),
        'sgu_ln_g': 1.0 + nrm(ks[8], (L, A_WIDTH), 0.02),
        'sgu_ln_b': nrm(ks[9], (L, A_WIDTH), 0.02),
        'sgu_w': nrm(ks[10], (L, A_GROUPS, SPATIAL_BLOCK, SPATIAL_BLOCK), SPATIAL_BLOCK ** -0.5),
        'sgu_b': 1.0 + nrm(ks[11], (L, A_GROUPS, SPATIAL_BLOCK), 0.02),
        'gla_norm_g': 1.0 + nrm(ks[12], (L, GLA_HEAD_V), 0.02),
        'w_branch_a': nrm(ks[13], (L, A_WIDTH, D), A_WIDTH ** -0.5),
        'w_branch_b': nrm(ks[14], (L, GLA_DV, D), GLA_DV ** -0.5),
        'w_out': nrm(ks[15], (L, D, D), D ** -0.5),
        'norm2_g': 1.0 + nrm(ks[16], (L, D), 0.02),
        'router_w': nrm(ks[17], (L, D, N_EXPERTS), D ** -0.5),
        'router_b': nrm(ks[18], (L, N_EXPERTS), 0.01),
        'exp_w_gate': nrm(ks[19], (L, N_EXPERTS, D, D_FF), D ** -0.5),
        'exp_b_gate': nrm(ks[20], (L, N_EXPERTS, D_FF), 0.01),
        'exp_w_up': nrm(ks[21], (L, N_EXPERTS, D, D_FF), D ** -0.5),
        'exp_b_up': nrm(ks[22], (L, N_EXPERTS, D_FF), 0.01),
        'exp_w_down': nrm(ks[23], (L, N_EXPERTS, D_FF, D), D_FF ** -0.5),
        'exp_b_down': nrm(ks[24], (L, N_EXPERTS, D), 0.01),
        'final_g': 1.0 + nrm(ks[25], (D,), 0.02),
    }


def reference(x, c, ada_w, ada_b, norm1_g, w_in, gla_gate_w2, gla_gate_b, sgu_ln_g, sgu_ln_b, sgu_w, sgu_b,
              gla_norm_g, w_branch_a, w_branch_b, w_out, norm2_g, router_w, router_b, exp_w_gate, exp_b_gate,
              exp_w_up, exp_b_up, exp_w_down, exp_b_down, final_g):
    B, S, D = x.shape
    cond = jax.nn.silu(c)
    split_points = _split_points()
    for l in range(DEPTH):
        mod = cond @ ada_w[l] + ada_b[l]
        sh1, sc1, gt1, sh2, sc2, gt2 = [m[:, None, :] for m in jnp.split(mod, 6, axis=-1)]

        h = rms_norm(x, norm1_g[l]) * (1.0 + sc1) + sh1
        proj = h @ w_in[l]
        a_u, a_v, q, k, v, r, g_lr, gate_a, gate_b = jnp.split(proj, split_points, axis=-1)
        y_a = spatial_gating(jax.nn.gelu(a_u), jax.nn.gelu(a_v), sgu_ln_g[l], sgu_ln_b[l], sgu_w[l], sgu_b[l])
        log_a = jax.nn.log_sigmoid((g_lr @ gla_gate_w2[l] + gla_gate_b[l]).astype(jnp.float32)) / GLA_GATE_TAU
        o = gated_linear_attention(q.reshape(B, S, GLA_HEADS, GLA_HEAD_K), k.reshape(B, S, GLA_HEADS, GLA_HEAD_K),
                                   v.reshape(B, S, GLA_HEADS, GLA_HEAD_V), log_a.reshape(B, S, GLA_HEADS, GLA_HEAD_K))
        o = rms_norm(o, gla_norm_g[l]).astype(x.dtype)
        y_b = o.reshape(B, S, GLA_DV) * jax.nn.silu(r)
        merged = jax.nn.sigmoid(gate_a) * (y_a @ w_branch_a[l]) + jax.nn.sigmoid(gate_b) * (y_b @ w_branch_b[l])
        x = x + gt1 * (merged @ w_out[l])

        h2 = rms_norm(x, norm2_g[l]) * (1.0 + sc2) + sh2
        x = x + gt2 * routed_experts(h2, router_w[l], router_b[l], exp_w_gate[l], exp_b_gate[l], exp_w_up[l],
                                     exp_b_up[l], exp_w_down[l], exp_b_down[l])
    return rms_norm(x, final_g)
```

```python
from contextlib import ExitStack

import numpy as np
import concourse.bass as bass
import concourse.mybir as mybir
from concourse.bass_utils import run_bass_kernel_spmd

ACT = mybir.ActivationFunctionType
ALU = mybir.AluOpType
F32 = mybir.dt.float32
BF16 = mybir.dt.bfloat16

P = 128
D = 2048
KC = 16
TOK = 1024
TT = 256
NSUB = TT // P
NTILE = TOK // TT
WB = 256
NWB = 6
NWB_X = 2
NE = 32
SEC_AU, SEC_AV, SEC_Q, SEC_K, SEC_V, SEC_R, SEC_GA, SEC_GB = 0, 8, 16, 20, 24, 32, 40, 48
EPS = 1e-6
GELU_A = 0.0713548162726
GELU_B = 1.5957691216057308


class Dep:
    __slots__ = ("w", "r")

    def __init__(self):
        self.w = None
        self.r = {}


class V:
    __slots__ = ("ap", "dep")

    def __init__(self, ap, dep=None):
        self.ap = ap
        self.dep = dep

    def __getitem__(self, idx):
        return V(self.ap[idx], self.dep)


class K:
    def __init__(self, nc, st):
        self.nc = nc
        self.st = st
        self.eng = {"pe": nc.tensor, "act": nc.scalar, "dve": nc.vector, "pool": nc.gpsimd, "sp": nc.sync}
        self.sem = {e: st.enter_context(nc.semaphore("sem_" + e)) for e in self.eng}
        self.cnt = {e: 0 for e in self.eng}
        self.waited = {e: {} for e in self.eng}
        self.dsem = {}
        self.dcnt = {}
        self.nps = 0

    def _sem_of(self, key):
        return self.sem[key] if key in self.sem else self.dsem[key]

    def _wait(self, e, tok):
        key, val = tok
        if self.waited[e].get(key, 0) >= val:
            return
        self.eng[e].wait_ge(self._sem_of(key), val)
        self.waited[e][key] = val

    def _pre(self, e, outs, ins):
        for v in ins:
            if v.dep is not None and v.dep.w is not None:
                self._wait(e, v.dep.w)
        for v in outs:
            if v.dep is None:
                continue
            if v.dep.w is not None:
                self._wait(e, v.dep.w)
            for key, val in v.dep.r.items():
                self._wait(e, (key, val))

    def _post(self, tok, outs, ins):
        for v in ins:
            if v.dep is not None:
                v.dep.r[tok[0]] = tok[1]
        for v in outs:
            if v.dep is not None:
                v.dep.w = tok
                v.dep.r = {}

    def op(self, e, build, outs=(), ins=()):
        self._pre(e, outs, ins)
        inst = build()
        self.cnt[e] += 1
        inst.then_inc(self.sem[e], 1)
        tok = (e, self.cnt[e])
        self._post(tok, outs, ins)
        return tok

    def pe(self, builds, outs, ins):
        self._pre("pe", outs, ins)
        inst = None
        for b in builds:
            inst = b()
        self.cnt["pe"] += 1
        inst.then_inc(self.sem["pe"], 1)
        tok = ("pe", self.cnt["pe"])
        self._post(tok, outs, ins)
        return tok

    def mm(self, out, pairs, extra_ins=()):
        n = len(pairs)
        nc = self.nc
        builds = [
            (lambda i=i, l=l, r=r: nc.tensor.matmul(out.ap, l.ap, r.ap, start=(i == 0), stop=(i == n - 1)))
            for i, (l, r) in enumerate(pairs)
        ]
        ins = [x for pr in pairs for x in pr] + list(extra_ins)
        return self.pe(builds, [out], ins)

    def dma(self, e, out, in_):
        self._pre(e, [out], [in_])
        d = out.dep if out.dep is not None else in_.dep
        key = ("d", id(d))
        if key not in self.dsem:
            self.dsem[key] = self.st.enter_context(self.nc.semaphore("dsem%d" % len(self.dsem)))
            self.dcnt[key] = 0
            self._keep = getattr(self, "_keep", [])
            self._keep.append(d)
        inst = self.eng[e].dma_start(out=out.ap, in_=in_.ap)
        self.dcnt[key] += 16
        inst.then_inc(self.dsem[key], 16)
        tok = (key, self.dcnt[key])
        self._post(tok, [out], [in_])
        return tok

    def wait_tok(self, e, tok):
        self._wait(e, tok)

    def barrier(self):
        toks = [(e, c) for e, c in self.cnt.items() if c > 0] + list(self.dcnt.items())
        for e in self.eng:
            for t in toks:
                if t[0] != e:
                    self._wait(e, t)


def _build(n_exp=NE, dbg=None, dbg_tiles=NTILE):
    nc = bass.Bass("TRN2", target_bir_lowering=False)
    st = ExitStack()
    k = K(nc, st)

    def din(name, shape, dt=F32):
        return V(nc.dram_tensor(name, list(shape), dt, kind="ExternalInput").ap(), None)

    xo = din("xo", [TOK, D])
    xp = din("xp", [TOK, D])
    flag_d = din("flag", [P, 1])
    cT_d = din("cT", [P, KC])
    adaw = din("adaw", [48, P, KC * WB])
    adabc_d = din("adabc", [P, 32])
    adabr_d = din("adabr", [1, 8192])
    g1_d = din("g1", [P, KC])
    win = din("win", [56, P, KC * WB])
    wglr_d = din("wglr", [P, KC * 16])
    w2aug_d = din("w2aug", [32, 1024])
    lng_d = din("lng", [P, D])
    lnb_d = din("lnb", [P, D])
    wmT_d = din("wmT", [P, 8 * P])
    sgub_d = din("sgub", [1, 8 * P])
    gn_d = din("gn", [P, 512])
    wa = din("wa", [8, P, KC * WB])
    wb = din("wb", [8, P, KC * WB])
    wo = din("wo", [8, P, KC * WB])
    n2g_d = din("n2g", [P, D])
    rw_d = din("rw", [P, KC * NE])
    rb_d = din("rb", [P, NE])
    fg_d = din("fg", [P, D])
    ident_d = din("ident", [P, P])
    tri_d = din("tri", [P, P])
    trirev_d = din("trirev", [P, P])
    cmask_d = din("cmask", [P, P])
    if n_exp > 0:
        ewg = din("ewg", [n_exp * 8, P, KC * WB])
        ewu = din("ewu", [n_exp * 8, P, KC * WB])
        ewd = din("ewd", [n_exp * 8, P, KC * WB])
        ebg_d = din("ebg", [P, NE * KC])
        ebu_d = din("ebu", [P, NE * KC])
        ebd_d = din("ebd", [NE, D])
    out_d = V(nc.dram_tensor("out", [TOK, D], F32, kind="ExternalOutput").ap(), Dep())
    x1_d = V(nc.dram_tensor("x1s", [TOK, D], F32, kind="Internal").ap(), Dep())
    dbg_d = None
    if dbg is not None:
        dbg_d = V(nc.dram_tensor("dbg", [TOK, D], F32, kind="ExternalOutput").ap(), Dep())

    cur = {"st": st}
    st_tm = ExitStack()

    def sb(name, shape, dt):
        t = cur["st"].enter_context(nc.sbuf_tensor("s_" + name, list(shape), dt))
        return V(t[:], Dep())

    def sbt(name, shape, dt):
        t = st_tm.enter_context(nc.sbuf_tensor("s_" + name, list(shape), dt))
        return V(t[:], Dep())

    def sb_view(v, dt, pattern=None, **kw):
        ap = v.ap if dt is None else v.ap.bitcast(dt)
        if pattern is not None:
            ap = ap.rearrange(pattern, **kw)
        return V(ap, v.dep)

    psf = []
    for i in range(7):
        t = st.enter_context(nc.psum_tensor("ps%d" % i, [P, 512], F32))
        psf.append(V(t[:], Dep()))
    tb = st.enter_context(nc.psum_tensor("psb", [P, 1024], BF16))
    psb = V(tb[:], Dep())

    def psum():
        v = psf[k.nps % 7]
        k.nps += 1
        return v

    ident_f = sb("ident_f", [P, P], F32)
    ident_b = sb("ident_b", [P, P], BF16)
    ones_row = sb("ones_row", [1, P], BF16)
    flag_sb = sb("flag_sb", [P, 1], F32)
    cT_sb = sb("cT_sb", [P, KC], F32)
    cond_f = sb("cond_f", [P, KC], F32)
    cond_b = sb("cond_b", [P, KC], BF16)
    adabc = sb("adabc", [P, 32], F32)
    adabr = sb("adabr", [1, WB], BF16)
    g1_sb = sb("g1_sb", [P, KC], F32)
    modc = sb("modc", [P, 32], F32)
    scale1 = sb("scale1", [P, KC], F32)
    small = sb("small", [P, 64], F32)
    wp_all = st.enter_context(nc.sbuf_tensor("s_wp_all", [P, NWB, KC, WB], BF16))
    wpool = [V(wp_all[:][:, i], Dep()) for i in range(NWB)]
    wstate = {"n": 0}
    wp_x = st_tm.enter_context(nc.sbuf_tensor("s_wp_x", [P, NWB_X, KC, WB], BF16))
    wpool += [V(wp_x[:][:, i], Dep()) for i in range(NWB_X)]
    tri_f = sbt("tri_f", [P, P], F32)
    trirev_f = sbt("trirev_f", [P, P], F32)
    cmask_f = sbt("cmask_f", [P, P], F32)
    wglr = sbt("wglr", [P, KC, 16], BF16)
    w2aug = sbt("w2aug", [32, 1024], F32)
    wmT = sbt("wmT", [P, 8, P], BF16)
    sgub = sbt("sgub", [1, 8 * P], BF16)
    gn_bc = sbt("gn_bc", [P, 512], F32)
    bcA = sbt("bcA", [P, D], F32)

    def sp_load(dst, src):
        return k.dma("sp", dst, src)

    sp_load(ident_f, ident_d)
    sp_load(tri_f, tri_d)
    sp_load(trirev_f, trirev_d)
    sp_load(cmask_f, cmask_d)
    sp_load(flag_sb, flag_d)
    sp_load(cT_sb, cT_d)
    sp_load(adabc, adabc_d)
    sp_load(g1_sb, g1_d)
    sp_load(w2aug, w2aug_d)
    sp_load(gn_bc, gn_d)
    k.dma("pool", sb_view(wglr, None, "p a b -> p (a b)"), wglr_d)
    k.dma("pool", sb_view(wmT, None, "p a b -> p (a b)"), wmT_d)
    k.dma("pool", sgub, sgub_d)
    k.op("dve", lambda: nc.vector.memset(ones_row.ap, 1.0), outs=[ones_row])
    k.op("dve", lambda: nc.vector.tensor_copy(out=ident_b.ap, in_=ident_f.ap), outs=[ident_b], ins=[ident_f])
    k.op("dve", lambda: nc.vector.memset(wmT.ap[64:128, :, 0:64], 0.0), outs=[wmT])

    def skey(v):
        return str(v.ap)

    wc_d = V(nc.dram_tensor("wcache", [80, P, KC * WB], BF16, kind="Internal").ap(), Dep())
    wcache = {}

    def cacheable(src):
        s = skey(src)
        return any(("'%s'" % nm) in s for nm in ("win", "wa", "wb", "wo"))

    def wload(src):
        buf = wpool[wstate["n"] % len(wpool)]
        wstate["n"] += 1
        flat = sb_view(buf, None, "p a b -> p (a b)")
        key = skey(src)
        if key in wcache and wcache[key][1]:
            k.dma("sp", flat, wc_d[wcache[key][0]])
        else:
            k.dma("pool", flat, src)
        return buf

    def wstore(src, buf):
        if not cacheable(src):
            return
        key = skey(src)
        if key in wcache:
            return
        idx = len(wcache)
        wcache[key] = [idx, False]
        k.dma("sp", wc_d[idx], sb_view(buf, None, "p a b -> p (a b)"))
        wcache[key][1] = True

    pf = []

    def stream(items, nxt=(), depth=None):
        depth = len(wpool) - 1 if depth is None else depth
        srcs = [it[0] for it in items] + list(nxt)[:depth]
        n = len(items)
        loaded = []
        for key, buf in pf:
            assert key == skey(srcs[len(loaded)]), "prefetch order mismatch"
            loaded.append(buf)
        del pf[:]

        def ensure(i):
            while len(loaded) <= i:
                loaded.append(wload(srcs[len(loaded)]))

        for i in range(min(depth, len(srcs))):
            ensure(i)
        for i in range(n):
            ensure(i)
            wstore(srcs[i], loaded[i])
            items[i][1](loaded[i])
            if i + depth < len(srcs):
                ensure(i + depth)
        for i in range(n, len(loaded)):
            pf.append((skey(srcs[i]), loaded[i]))

    k.op("act", lambda: nc.scalar.activation(out=cond_f.ap, in_=cT_sb.ap, func=ACT.Silu), outs=[cond_f], ins=[cT_sb])
    k.op("dve", lambda: nc.vector.tensor_copy(out=cond_b.ap, in_=cond_f.ap), outs=[cond_b], ins=[cond_f])
    cr = {}

    def build_condrep(tag):
        ones_f = sb("ones_f" + tag, [P, P], F32)
        crep = sb("condrep" + tag, [P, KC, P], BF16)
        k.op("dve", lambda: nc.vector.memset(ones_f.ap, 1.0), outs=[ones_f])
        for kk in range(KC):
            k.op("dve", lambda kk=kk: nc.vector.tensor_scalar(out=crep.ap[:, kk, :], in0=ones_f.ap,
                                                             scalar1=cond_f.ap[:, kk:kk + 1], scalar2=None, op0=ALU.mult),
                 outs=[crep], ins=[ones_f, cond_f])
        cr["v"] = crep

    psA = psum()

    def modcol_item(bi):
        def fn(buf):
            for jj in range(2):
                j = bi * 2 + jj
                k.mm(psA[:, j:j + 1], [(buf[:, kk, jj * P:(jj + 1) * P], cond_b[:, kk:kk + 1]) for kk in range(KC)])
        return (adaw[bi], fn)

    def modrow_seg(seg, dst, post=None):
        items = []
        for bb in range(8):
            bi = seg * 8 + bb

            def fn(buf, bb=bb):
                ps = psum()
                c0 = (seg - 2) * D + bb * WB
                k.dma("pool", adabr, adabr_d[:, c0:c0 + WB])
                pairs = [(cr["v"][:, kk, :], buf[:, kk, :]) for kk in range(KC)]
                pairs.append((ones_row[0:1, :], adabr[0:1, :]))
                k.mm(ps[:, 0:WB], pairs)
                k.op("act", lambda: nc.scalar.copy(out=dst.ap[:, bb * WB:(bb + 1) * WB], in_=ps.ap[:, 0:WB]),
                     outs=[dst], ins=[ps])
            items.append((adaw[bi], fn))
        return items

    stream([modcol_item(bi) for bi in range(16)])
    k.op("dve", lambda: nc.vector.tensor_tensor(out=modc.ap, in0=psA.ap[:, 0:32], in1=adabc.ap, op=ALU.add),
         outs=[modc], ins=[psA, adabc])
    shift1 = modc
    k.op("dve", lambda: nc.vector.scalar_tensor_tensor(out=scale1.ap, in0=modc.ap[:, 16:32], scalar=1.0, in1=g1_sb.ap,
                                                      op0=ALU.add, op1=ALU.mult),
         outs=[scale1], ins=[modc, g1_sb])
    cur["st"] = st_tm
    build_condrep("_tm")
    cur["st"] = st
    stream(modrow_seg(2, bcA))

    cur["st"] = st_tm
    xs = sb("xs", [P, D], F32)
    hT = sb("hT", [P, KC, TT], BF16)
    lbuf = sb("lbuf", [P, NSUB, 1024], F32)
    ks = sb("ks", [P, NSUB, 1024], BF16)
    vtok = sb("vtok", [P, NSUB, D], BF16)
    qdT = sb("qdT", [P, 8, TT], BF16)
    kiT = sb("kiT", [P, 8, TT], BF16)
    sr = sb("sr", [P, NSUB, D], BF16)
    yb = sb("yb", [P, D], BF16)
    ybT = sb("ybT", [P, KC, TT], BF16)
    glrT = sb("glrT", [32, TT], F32)
    dT = sb("dT", [P, 8, NSUB], F32)
    S32 = sb("S32", [P, 8, 512], F32)
    Sbf = sb("Sbf", [P, 8, 512], BF16)
    t_all = cur["st"].enter_context(nc.sbuf_tensor("s_t_all", [P, 8, 256], F32))
    tpool = [V(t_all[:][:, i, :], Dep()) for i in range(8)]
    tstate = {"n": 0}

    def tmp():
        v = tpool[tstate["n"] % 8]
        tstate["n"] += 1
        return v

    attm2 = [sb("attm%d" % i, [P, P], BF16) for i in range(2)]
    lnbc = sb("lnbc", [P, 2, D], BF16)
    stat = sb("stat", [P, 64], F32)
    guT = sb_view(xs, BF16, "p (a b) -> p a b", a=KC)[:, :, 0:TT]
    gv = sb("gv", [P, NSUB, D], BF16)
    sgaT = sb_view(vtok, None, "p s (a b) -> p (s a) b", b=TT)
    sgbT = sb_view(sr, None, "p s (a b) -> p (s a) b", b=TT)

    k.dma("pool", lnbc[:, 0, :], lng_d)
    k.dma("pool", lnbc[:, 1, :], lnb_d)
    k.op("dve", lambda: nc.vector.memset(glrT.ap, 1.0), outs=[glrT])
    k.op("dve", lambda: nc.vector.memset(S32.ap, 0.0), outs=[S32])
    k.op("dve", lambda: nc.vector.memset(Sbf.ap, 0.0), outs=[Sbf])

    def rstd_from_ssq(dst, ssq, n):
        k.op("dve", lambda: nc.vector.tensor_scalar(out=dst.ap, in0=ssq.ap, scalar1=1.0 / n, scalar2=EPS,
                                                   op0=ALU.mult, op1=ALU.add), outs=[dst], ins=[ssq])
        k.op("act", lambda: nc.scalar.activation(out=dst.ap, in_=dst.ap, func=ACT.Ln), outs=[dst], ins=[dst])
        k.op("act", lambda: nc.scalar.activation(out=dst.ap, in_=dst.ap, func=ACT.Exp, scale=-0.5), outs=[dst], ins=[dst])

    def gelu(ps_v, out_v, n, accum=None):
        a = tmp()[:, 0:n]
        b = tmp()[:, 0:n]
        k.op("act", lambda: nc.scalar.activation(out=a.ap, in_=ps_v.ap, func=ACT.Square), outs=[a], ins=[ps_v])
        k.op("dve", lambda: nc.vector.tensor_scalar(out=a.ap, in0=a.ap, scalar1=GELU_A, scalar2=GELU_B,
                                                   op0=ALU.mult, op1=ALU.add), outs=[a], ins=[a])
        k.op("dve", lambda: nc.vector.tensor_tensor(out=a.ap, in0=a.ap, in1=ps_v.ap, op=ALU.mult), outs=[a], ins=[a, ps_v])
        k.op("act", lambda: nc.scalar.activation(out=b.ap, in_=a.ap, func=ACT.Sigmoid), outs=[b], ins=[a])
        if accum is None:
            k.op("dve", lambda: nc.vector.tensor_tensor(out=out_v.ap, in0=b.ap, in1=ps_v.ap, op=ALU.mult),
                 outs=[out_v], ins=[b, ps_v])
        else:
            k.op("dve", lambda: nc.vector.scalar_tensor_tensor(out=out_v.ap, in0=b.ap, scalar=1.0, in1=ps_v.ap,
                                                              op0=ALU.mult, op1=ALU.mult, accum_out=accum.ap),
                 outs=[out_v, accum], ins=[b, ps_v])

    def srcs_s1(prefix):
        if prefix:
            return [win[SEC_K + b] for b in range(4)] + [win[SEC_V + b] for b in range(8)]
        out = [win[SEC_V + b] for b in range(8)]
        for b in range(4):
            out += [win[SEC_Q + b], win[SEC_K + b]]
        return out + [win[SEC_R + b] for b in range(8)]

    def srcs_s2():
        return [win[SEC_AU + b] for b in range(8)] + [win[SEC_AV + b] for b in range(8)]

    def srcs_s3():
        return [win[SEC_GA + b] for b in range(8)] + [win[SEC_GB + b] for b in range(8)] + [wa[b] for b in range(8)]

    def prep(xsrc, t0):
        for s in range(NSUB):
            sp_load(xs, xsrc[t0 + s * P:t0 + (s + 1) * P, :])
            ssq = small[:, 0:1]
            rs = small[:, 1:2]
            k.op("act", lambda: nc.scalar.activation(out=yb.ap, in_=xs.ap, func=ACT.Square, accum_out=ssq.ap),
                 outs=[yb, ssq], ins=[xs])
            rstd_from_ssq(rs, ssq, D)
            k.op("dve", lambda: nc.vector.tensor_scalar(out=xs.ap, in0=xs.ap, scalar1=rs.ap, scalar2=None,
                                                       op0=ALU.mult), outs=[xs], ins=[xs, rs])
            yield
            for kq in range(4):
                ps = psum()
                k.pe([(lambda kk=kk: nc.tensor.transpose(out=ps.ap[:, kk * P:(kk + 1) * P],
                                                        in_=xs.ap[:, (kq * 4 + kk) * P:(kq * 4 + kk + 1) * P],
                                                        identity=ident_f.ap)) for kk in range(4)],
                     outs=[ps], ins=[xs, ident_f])
                for kk in range(4):
                    kc = kq * 4 + kk
                    if kk % 2 == 0:
                        k.op("act", lambda kk=kk, kc=kc: nc.scalar.activation(
                            out=hT.ap[:, kc, s * P:(s + 1) * P], in_=ps.ap[:, kk * P:(kk + 1) * P], func=ACT.Identity,
                            scale=scale1.ap[:, kc:kc + 1], bias=shift1.ap[:, kc:kc + 1]),
                            outs=[hT], ins=[ps, scale1, shift1])
                    else:
                        k.op("dve", lambda kk=kk, kc=kc: nc.vector.tensor_scalar(
                            out=hT.ap[:, kc, s * P:(s + 1) * P], in0=ps.ap[:, kk * P:(kk + 1) * P],
                            scalar1=scale1.ap[:, kc:kc + 1], scalar2=shift1.ap[:, kc:kc + 1], op0=ALU.mult, op1=ALU.add),
                            outs=[hT], ins=[ps, scale1, shift1])

        yield
        ps = psum()
        k.mm(ps[0:16, 0:TT], [(wglr[:, kk, :], hT[:, kk, :]) for kk in range(KC)])
        k.op("act", lambda: nc.scalar.copy(out=glrT.ap[0:16, :], in_=ps.ap[0:16, 0:TT]), outs=[glrT], ins=[ps])
        for s in range(NSUB):
            for nb in range(4):
                ps = psum()
                k.mm(ps[:, 0:WB], [(glrT[0:32, s * P:(s + 1) * P], w2aug[0:32, nb * WB:(nb + 1) * WB])])
                tz = tmp()
                k.op("act", lambda: nc.scalar.activation(out=tz.ap, in_=ps.ap[:, 0:WB], func=ACT.Exp, scale=-1.0),
                     outs=[tz], ins=[ps])
                k.op("act", lambda: nc.scalar.activation(out=lbuf.ap[:, s, nb * WB:(nb + 1) * WB], in_=tz.ap,
                                                        func=ACT.Ln, bias=1.0), outs=[lbuf], ins=[tz])


    def token_tile(xsrc, t0, prefix, nxt_tile=(), do_prep=True, next_prep=None):
        if do_prep:
            for _ in prep(xsrc, t0):
                pass

        items = []

        def ks_block(buf, bi):
            if True:
                for s in range(NSUB):
                    cols = slice(bi * WB, (bi + 1) * WB)
                    psr = psum()
                    k.mm(psr[:, 0:WB], [(trirev_f, lbuf[:, s, cols])])
                    ter = tmp()
                    k.op("act", lambda: nc.scalar.activation(out=ter.ap, in_=psr.ap[:, 0:WB], func=ACT.Exp),
                         outs=[ter], ins=[psr])
                    psk = psum()
                    k.mm(psk[:, 0:WB], [(hT[:, kk, s * P:(s + 1) * P], buf[:, kk, :]) for kk in range(KC)])
                    k.op("dve", lambda: nc.vector.tensor_tensor(out=ks.ap[:, s, cols], in0=psk.ap[:, 0:WB],
                                                               in1=ter.ap, op=ALU.mult),
                         outs=[ks], ins=[psk, ter])

        if prefix:
            for bi in range(4):
                items.append((win[SEC_K + bi], (lambda buf, bi=bi: ks_block(buf, bi))))
        for bi in range(8):
            def fn(buf, bi=bi):
                for s in range(NSUB):
                    psv = psum()
                    k.mm(psv[:, 0:WB], [(hT[:, kk, s * P:(s + 1) * P], buf[:, kk, :]) for kk in range(KC)])
                    k.op("act", lambda: nc.scalar.copy(out=vtok.ap[:, s, bi * WB:(bi + 1) * WB], in_=psv.ap[:, 0:WB]),
                         outs=[vtok], ins=[psv])
            items.append((win[SEC_V + bi], fn))

        def bT_chunk(j, need_exp):
            pst = psum()
            for s in range(NSUB):
                k.mm(pst[:, s * P:(s + 1) * P], [(lbuf[:, s, j * P:(j + 1) * P], tri_f)])
            for s in range(NSUB):
                k.op("act", lambda s=s: nc.scalar.activation(out=dT.ap[:, j, s:s + 1],
                                                            in_=pst.ap[:, s * P + P - 1:s * P + P], func=ACT.Exp),
                     outs=[dT], ins=[pst])
            if need_exp:
                teb = tmp()
                ten = tmp()
                k.op("act", lambda: nc.scalar.activation(out=teb.ap, in_=pst.ap[:, 0:TT], func=ACT.Exp),
                     outs=[teb], ins=[pst])
                k.op("act", lambda: nc.scalar.activation(out=ten.ap, in_=pst.ap[:, 0:TT], func=ACT.Exp, scale=-1.0),
                     outs=[ten], ins=[pst])
                return teb, ten
            return None, None

        if prefix:
            for j in range(8):
                bT_chunk(j, False)
        else:
            for bi in range(4):
                holder = {}

                def fq(buf, bi=bi, holder=holder):
                    holder["q"] = buf

                def fk(buf, bi=bi, holder=holder):
                    qb = holder["q"]
                    for jj in range(2):
                        j = bi * 2 + jj
                        teb, ten = bT_chunk(j, True)
                        psq = psum()
                        k.mm(psq[:, 0:TT], [(qb[:, kk, jj * P:(jj + 1) * P], hT[:, kk, :]) for kk in range(KC)])
                        k.op("dve", lambda: nc.vector.scalar_tensor_tensor(out=qdT.ap[:, j, :], in0=psq.ap[:, 0:TT], scalar=0.0625,
                                                                          in1=teb.ap, op0=ALU.mult, op1=ALU.mult),
                             outs=[qdT], ins=[psq, teb])
                        psk = psum()
                        k.mm(psk[:, 0:TT], [(buf[:, kk, jj * P:(jj + 1) * P], hT[:, kk, :]) for kk in range(KC)])
                        k.op("dve", lambda: nc.vector.tensor_tensor(out=kiT.ap[:, j, :], in0=psk.ap[:, 0:TT], in1=ten.ap,
                                                                   op=ALU.mult), outs=[kiT], ins=[psk, ten])
                    ks_block(buf, bi)
                items.append((win[SEC_Q + bi], fq))
                items.append((win[SEC_K + bi], fk))
            for bi in range(8):
                def fn(buf, bi=bi):
                    for s in range(NSUB):
                        psr = psum()
                        k.mm(psr[:, 0:WB], [(hT[:, kk, s * P:(s + 1) * P], buf[:, kk, :]) for kk in range(KC)])
                        tsr = tmp()
                        k.op("act", lambda: nc.scalar.activation(out=tsr.ap, in_=psr.ap[:, 0:WB], func=ACT.Silu),
                             outs=[tsr], ins=[psr])
                        gc = (bi * WB) % 512
                        k.op("dve", lambda: nc.vector.tensor_tensor(out=sr.ap[:, s, bi * WB:(bi + 1) * WB], in0=tsr.ap,
                                                                   in1=gn_bc.ap[:, gc:gc + WB], op=ALU.mult),
                             outs=[sr], ins=[tsr, gn_bc])
                items.append((win[SEC_R + bi], fn))
        stream(items, nxt=(nxt_tile if prefix else srcs_s2()))
        if prefix and next_prep is not None:
            next(next_prep, None)

        for s in range(NSUB):
            tcols = slice(s * P, (s + 1) * P)
            if not prefix:
                for hd in range(4):
                    psa = psum()
                    k.mm(psa[:, 0:P], [(kiT[:, hd * 2 + kc, tcols], qdT[:, hd * 2 + kc, tcols]) for kc in range(2)])
                    attm = attm2[hd % 2]
                    k.op("dve", lambda: nc.vector.tensor_tensor(out=attm.ap, in0=psa.ap[:, 0:P], in1=cmask_f.ap, op=ALU.mult),
                         outs=[attm], ins=[psa, cmask_f])
                    pso = psum()
                    k.mm(pso[:, 0:512], [(attm, vtok[:, s, hd * 512:(hd + 1) * 512]),
                                         (qdT[:, hd * 2, tcols], Sbf[:, hd * 2, :]),
                                         (qdT[:, hd * 2 + 1, tcols], Sbf[:, hd * 2 + 1, :])])
                    ssq = small[:, 16 + 2 * hd:17 + 2 * hd]
                    rs = small[:, 17 + 2 * hd:18 + 2 * hd]
                    k.op("act", lambda hd=hd: nc.scalar.activation(out=yb.ap[:, hd * 512:(hd + 1) * 512], in_=pso.ap, func=ACT.Square,
                                                                  accum_out=ssq.ap), outs=[yb, ssq], ins=[pso])
                    rstd_from_ssq(rs, ssq, 512)
                    k.op("dve", lambda hd=hd: nc.vector.scalar_tensor_tensor(
                        out=yb.ap[:, hd * 512:(hd + 1) * 512], in0=pso.ap, scalar=rs.ap, in1=sr.ap[:, s, hd * 512:(hd + 1) * 512],
                        op0=ALU.mult, op1=ALU.mult), outs=[yb], ins=[pso, rs, sr])
                for half in range(2):
                    k.pe([(lambda kk=kk: nc.tensor.transpose(out=psb.ap[:, kk * P:(kk + 1) * P],
                                                            in_=yb.ap[:, (half * 8 + kk) * P:(half * 8 + kk + 1) * P],
                                                            identity=ident_b.ap)) for kk in range(8)],
                         outs=[psb], ins=[yb, ident_b])
                    k.op("act", lambda half=half: nc.scalar.copy(
                        out=ybT.ap[:, half * 8:(half + 1) * 8, tcols],
                        in_=psb.ap.rearrange("p (a b) -> p a b", a=8)), outs=[ybT], ins=[psb])
            for hd in range(4):
                if prefix and next_prep is not None and hd in (0, 2) and not (s == 0 and hd == 0):
                    next(next_prep, None)
                for kc in range(2):
                    idx = hd * 2 + kc
                    pss = psum()
                    k.mm(pss[:, 0:512], [(ks[:, s, idx * P:(idx + 1) * P], vtok[:, s, hd * 512:(hd + 1) * 512])])
                    k.op("dve", lambda idx=idx: nc.vector.scalar_tensor_tensor(
                        out=S32.ap[:, idx, :], in0=S32.ap[:, idx, :], scalar=dT.ap[:, idx, s:s + 1], in1=pss.ap,
                        op0=ALU.mult, op1=ALU.add), outs=[S32], ins=[S32, dT, pss])
                    k.op("act", lambda idx=idx: nc.scalar.copy(out=Sbf.ap[:, idx, :], in_=S32.ap[:, idx, :]),
                         outs=[Sbf], ins=[S32])
        if prefix:
            if next_prep is not None:
                for _ in next_prep:
                    pass
            return

        items = []
        for bi in range(8):
            def fn(buf, bi=bi):
                for jj in range(2):
                    j = bi * 2 + jj
                    psu = psum()
                    k.mm(psu[:, 0:TT], [(buf[:, kk, jj * P:(jj + 1) * P], hT[:, kk, :]) for kk in range(KC)])
                    gelu(psu[:, 0:TT], guT[:, j, :], TT)
            items.append((win[SEC_AU + bi], fn))
        for bi in range(8):
            def fn(buf, bi=bi):
                for s in range(NSUB):
                    psv = psum()
                    k.mm(psv[:, 0:WB], [(hT[:, kk, s * P:(s + 1) * P], buf[:, kk, :]) for kk in range(KC)])
                    gelu(psv[:, 0:WB], gv[:, s, bi * WB:(bi + 1) * WB], WB, accum=stat[:, s * 16 + bi:s * 16 + bi + 1])
                    tj = tmp()
                    k.op("act", lambda: nc.scalar.activation(out=tj.ap, in_=gv.ap[:, s, bi * WB:(bi + 1) * WB],
                                                            func=ACT.Square, accum_out=stat.ap[:, s * 16 + 8 + bi:s * 16 + 9 + bi]),
                         outs=[tj, stat], ins=[gv])
            items.append((win[SEC_AV + bi], fn))
        stream(items, nxt=srcs_s3())
        for s in range(NSUB):
            mean = small[:, 4:5]
            ex2 = small[:, 5:6]
            var = small[:, 6:7]
            rs = small[:, 7:8]
            k.op("dve", lambda: nc.vector.reduce_sum(out=mean.ap, in_=stat.ap[:, s * 16:s * 16 + 8], axis=mybir.AxisListType.X),
                 outs=[mean], ins=[stat])
            k.op("dve", lambda: nc.vector.reduce_sum(out=ex2.ap, in_=stat.ap[:, s * 16 + 8:s * 16 + 16], axis=mybir.AxisListType.X),
                 outs=[ex2], ins=[stat])
            k.op("dve", lambda: nc.vector.tensor_scalar(out=mean.ap, in0=mean.ap, scalar1=1.0 / D, scalar2=None, op0=ALU.mult),
                 outs=[mean], ins=[mean])
            k.op("dve", lambda: nc.vector.tensor_tensor(out=var.ap, in0=mean.ap, in1=mean.ap, op=ALU.mult), outs=[var], ins=[mean])
            k.op("dve", lambda: nc.vector.scalar_tensor_tensor(out=var.ap, in0=ex2.ap, scalar=1.0 / D, in1=var.ap,
                                                              op0=ALU.mult, op1=ALU.subtract), outs=[var], ins=[ex2, var])
            rstd_from_ssq(rs, var, 1.0)
            k.op("dve", lambda: nc.vector.tensor_scalar(out=gv.ap[:, s, :], in0=gv.ap[:, s, :], scalar1=mean.ap, scalar2=rs.ap,
                                                       op0=ALU.subtract, op1=ALU.mult), outs=[gv], ins=[gv, mean, rs])
            k.op("dve", lambda: nc.vector.tensor_tensor(out=gv.ap[:, s, :], in0=gv.ap[:, s, :], in1=lnbc.ap[:, 0, :], op=ALU.mult),
                 outs=[gv], ins=[gv, lnbc])
            k.op("dve", lambda: nc.vector.tensor_tensor(out=gv.ap[:, s, :], in0=gv.ap[:, s, :], in1=lnbc.ap[:, 1, :], op=ALU.add),
                 outs=[gv], ins=[gv, lnbc])
            for jq in range(4):
                psm = psum()
                for jj in range(4):
                    j = jq * 4 + jj
                    g = j // 2
                    k.mm(psm[:, jj * P:(jj + 1) * P], [(gv[:, s, j * P:(j + 1) * P], wmT[:, g, :]),
                                                        (ones_row[0:1, :], sgub[0:1, g * P:(g + 1) * P])])
                k.op("dve", lambda jq=jq: nc.vector.tensor_tensor(
                    out=guT.ap[:, jq * 4:(jq + 1) * 4, s * P:(s + 1) * P], in0=guT.ap[:, jq * 4:(jq + 1) * 4, s * P:(s + 1) * P],
                    in1=psm.ap.rearrange("p (a b) -> p a b", a=4), op=ALU.mult), outs=[guT], ins=[guT, psm])
        yaT = guT

        items = []
        for bi in range(8):
            def fn(buf, bi=bi):
                for jj in range(2):
                    m = bi * 2 + jj
                    psg = psum()
                    k.mm(psg[:, 0:TT], [(buf[:, kk, jj * P:(jj + 1) * P], hT[:, kk, :]) for kk in range(KC)])
                    k.op("act", lambda: nc.scalar.activation(out=sgaT.ap[:, m, :], in_=psg.ap[:, 0:TT], func=ACT.Sigmoid),
                         outs=[sgaT], ins=[psg])
            items.append((win[SEC_GA + bi], fn))
        for bi in range(8):
            def fn(buf, bi=bi):
                for jj in range(2):
                    m = bi * 2 + jj
                    psg = psum()
                    k.mm(psg[:, 0:TT], [(buf[:, kk, jj * P:(jj + 1) * P], hT[:, kk, :]) for kk in range(KC)])
                    k.op("act", lambda: nc.scalar.activation(out=sgbT.ap[:, m, :], in_=psg.ap[:, 0:TT], func=ACT.Sigmoid),
                         outs=[sgbT], ins=[psg])
            items.append((win[SEC_GB + bi], fn))
        for bi in range(8):
            def fn(buf, bi=bi):
                for jj in range(2):
                    m = bi * 2 + jj
                    psp = psum()
                    k.mm(psp[:, 0:TT], [(buf[:, kk, jj * P:(jj + 1) * P], yaT[:, kk, :]) for kk in range(KC)])
                    k.op("dve", lambda: nc.vector.tensor_tensor(out=sgaT.ap[:, m, :], in0=psp.ap[:, 0:TT], in1=sgaT.ap[:, m, :],
                                                               op=ALU.mult), outs=[sgaT], ins=[psp, sgaT])
            items.append((wa[bi], fn))
        for bi in range(8):
            def fn(buf, bi=bi):
                if bi in (0, 3, 6) and next_prep is not None:
                    next(next_prep, None)
                for jj in range(2):
                    m = bi * 2 + jj
                    psp = psum()
                    k.mm(psp[:, 0:TT], [(buf[:, kk, jj * P:(jj + 1) * P], ybT[:, kk, :]) for kk in range(KC)])
                    k.op("dve", lambda: nc.vector.tensor_tensor(out=sgbT.ap[:, m, :], in0=psp.ap[:, 0:TT], in1=sgbT.ap[:, m, :],
                                                               op=ALU.mult), outs=[sgbT], ins=[psp, sgbT])
                    k.op("dve", lambda: nc.vector.tensor_tensor(out=sgbT.ap[:, m, :], in0=sgbT.ap[:, m, :], in1=sgaT.ap[:, m, :],
                                                                op=ALU.add), outs=[sgbT], ins=[sgbT, sgaT])
            items.append((wb[bi], fn))
        mT = sgbT
        for bi in range(8):
            def fn(buf, bi=bi):
                if bi == 1 and next_prep is not None:
                    for _ in next_prep:
                        pass
                for s in range(NSUB):
                    pso = psum()
                    k.mm(pso[:, 0:WB], [(mT[:, kk, s * P:(s + 1) * P], buf[:, kk, :]) for kk in range(KC)])
                    cols = slice(bi * WB, (bi + 1) * WB)
                    rows = slice(t0 + s * P, t0 + (s + 1) * P)
                    xpc = tmp()
                    sp_load(xpc, xsrc[rows, cols])
                    to = tmp()
                    k.op("dve", lambda: nc.vector.tensor_tensor(out=to.ap, in0=pso.ap[:, 0:WB], in1=bcA.ap[:, cols],
                                                               op=ALU.mult), outs=[to], ins=[pso, bcA])
                    k.op("dve", lambda: nc.vector.tensor_tensor(out=xpc.ap, in0=xpc.ap, in1=to.ap, op=ALU.add),
                         outs=[xpc], ins=[xpc, to])
                    sp_load(x1_d[rows, cols], xpc)
            items.append((wo[bi], fn))
        stream(items, nxt=nxt_tile)


    n_pref = NTILE if dbg != "noprefix" else 0
    mstage = int(dbg[1:]) if (dbg is not None and dbg.startswith("m")) else 99
    n_main = NTILE if (dbg is None or mstage != 99) else int(dbg_tiles)
    tiles = [(xp, ti * TT, True) for ti in range(n_pref)] + [(xo, ti * TT, False) for ti in range(n_main)]
    for i, (xsrc_, t0_, pre_) in enumerate(tiles):
        nx = tiles[i + 1] if i + 1 < len(tiles) else None
        if i == n_pref and n_pref > 0:
            pass
        token_tile(xsrc_, t0_, pre_,
                   nxt_tile=(srcs_s1(nx[2]) if nx is not None else ()),
                   do_prep=(i == 0),
                   next_prep=(prep(nx[0], nx[1]) if nx is not None else None))
        if pre_ and (nx is None or not nx[2]):
            k.op("dve", lambda: nc.vector.tensor_scalar(out=S32.ap, in0=S32.ap, scalar1=flag_sb.ap, scalar2=None, op0=ALU.mult),
                 outs=[S32], ins=[S32, flag_sb])
            k.op("act", lambda: nc.scalar.copy(out=Sbf.ap, in_=S32.ap), outs=[Sbf], ins=[S32])
    k.barrier()
    st_tm.close()
    cur["st"] = st
    del wpool[NWB:]
    wstate["n"] = 0

    def dbg_exit():
        tok = k.dma("sp", dbg_d, x1_d)
        k.wait_tok("sp", tok)
        return nc, st

    if dbg is not None and mstage == 99:
        return dbg_exit()

    acc = sb("acc", [P, TOK // P, D], F32)
    h2T = sb("h2T", [P, KC, TOK], BF16)
    gt2 = sb("gt2", [P, D], F32)
    gates = sb("gates", [P, TOK // P, NE], F32)
    GT = sb("GT", [NE, TOK], BF16)
    ebg = sb("ebg", [P, NE * KC], F32)
    ebu = sb("ebu", [P, NE * KC], F32)
    ebd = sb("ebd", [NE, D], BF16)
    rw = sb("rw", [P, KC, NE], F32)
    rb = sb("rb", [P, NE], F32)
    m_all = st.enter_context(nc.sbuf_tensor("s_m_all", [P, 4, 512], F32))
    mA, mB, mC, mD = [V(m_all[:][:, i, :], Dep()) for i in range(4)]
    sp_load(sb_view(rw, None, "p a b -> p (a b)"), rw_d)
    sp_load(rb, rb_d)
    if n_exp > 0:
        sp_load(ebg, ebg_d)
        sp_load(ebu, ebu_d)
        k.dma("pool", ebd, ebd_d)

    if mstage == 1:
        return dbg_exit()
    st_m1 = ExitStack()
    cur["st"] = st_m1
    sh2 = sb("sh2", [P, D], F32)
    A2 = sb("A2", [P, D], F32)
    h2f = sb("h2f", [P, D], F32)
    h2Tf = V(m_all[:].rearrange("p a (b c) -> p (a b) c", c=P), Dep())
    rt = sb("rt", [P, 8 * NE], F32)
    build_condrep("_m1")
    sp_load(h2f, n2g_d)
    stream(modrow_seg(3, sh2))
    stream(modrow_seg(4, A2))
    k.op("dve", lambda: nc.vector.scalar_tensor_tensor(out=A2.ap, in0=A2.ap, scalar=1.0, in1=h2f.ap, op0=ALU.add, op1=ALU.mult),
         outs=[A2], ins=[A2, h2f])
    stream(modrow_seg(5, gt2))
    if mstage == 2:
        return dbg_exit()
    for sub in range(TOK // P):
        tcols = slice(sub * P, (sub + 1) * P)
        sp_load(acc[:, sub, :], x1_d[sub * P:(sub + 1) * P, :])
        if mstage == 31:
            continue
        ssq = small[:, 8:9]
        rs = small[:, 9:10]
        k.op("act", lambda: nc.scalar.activation(out=h2f.ap, in_=acc.ap[:, sub, :], func=ACT.Square, accum_out=ssq.ap),
             outs=[h2f, ssq], ins=[acc])
        rstd_from_ssq(rs, ssq, D)
        if mstage == 32:
            continue
        k.op("dve", lambda: nc.vector.scalar_tensor_tensor(out=h2f.ap, in0=acc.ap[:, sub, :], scalar=rs.ap, in1=A2.ap,
                                                          op0=ALU.mult, op1=ALU.mult), outs=[h2f], ins=[acc, rs, A2])
        k.op("dve", lambda: nc.vector.tensor_tensor(out=h2f.ap, in0=h2f.ap, in1=sh2.ap, op=ALU.add), outs=[h2f], ins=[h2f, sh2])
        if mstage == 33:
            continue
        for kq in range(4):
            ps = psum()
            k.pe([(lambda kk=kk: nc.tensor.transpose(out=ps.ap[:, kk * P:(kk + 1) * P],
                                                    in_=h2f.ap[:, (kq * 4 + kk) * P:(kq * 4 + kk + 1) * P],
                                                    identity=ident_f.ap)) for kk in range(4)], outs=[ps], ins=[h2f, ident_f])
            k.op("act", lambda kq=kq: nc.scalar.copy(out=h2Tf.ap[:, kq * 4:(kq + 1) * 4, :],
                                                    in_=ps.ap.rearrange("p (a b) -> p a b", a=4)), outs=[h2Tf], ins=[ps])
            if mstage == 34:
                continue
            k.op("act", lambda kq=kq: nc.scalar.copy(out=h2T.ap[:, kq * 4:(kq + 1) * 4, tcols],
                                                    in_=ps.ap.rearrange("p (a b) -> p a b", a=4)), outs=[h2T], ins=[ps])
        if mstage in (3, 31, 32, 33, 34):
            continue
        psr = psum()
        k.mm(psr[:, 0:NE], [(h2Tf[:, kk, :], rw[:, kk, :]) for kk in range(KC)])
        lg = rt[:, 0:NE]
        wk = rt[:, NE:2 * NE]
        sel = rt[:, 2 * NE:3 * NE]
        eq = rt[:, 3 * NE:4 * NE]
        ex = rt[:, 4 * NE:5 * NE]
        mx = small[:, 10:11]
        m1 = small[:, 11:12]
        sm = small[:, 12:13]
        k.op("dve", lambda: nc.vector.tensor_tensor(out=lg.ap, in0=psr.ap[:, 0:NE], in1=rb.ap, op=ALU.add), outs=[lg], ins=[psr, rb])
        k.op("dve", lambda: nc.vector.tensor_copy(out=wk.ap, in_=lg.ap), outs=[wk], ins=[lg])
        k.op("dve", lambda: nc.vector.memset(sel.ap, 0.0), outs=[sel])
        for r_ in range(4):
            dst = m1 if r_ == 0 else mx
            k.op("dve", lambda dst=dst: nc.vector.reduce_max(out=dst.ap, in_=wk.ap, axis=mybir.AxisListType.X), outs=[dst], ins=[wk])
            k.op("dve", lambda dst=dst: nc.vector.tensor_scalar(out=eq.ap, in0=wk.ap, scalar1=dst.ap, scalar2=None, op0=ALU.is_equal),
                 outs=[eq], ins=[wk, dst])
            k.op("dve", lambda: nc.vector.tensor_tensor(out=sel.ap, in0=sel.ap, in1=eq.ap, op=ALU.add), outs=[sel], ins=[sel, eq])
            k.op("dve", lambda: nc.vector.scalar_tensor_tensor(out=wk.ap, in0=eq.ap, scalar=-1e30, in1=wk.ap, op0=ALU.mult, op1=ALU.add),
                 outs=[wk], ins=[eq, wk])
        k.op("dve", lambda: nc.vector.tensor_scalar(out=m1.ap, in0=m1.ap, scalar1=-1.0, scalar2=None, op0=ALU.mult), outs=[m1], ins=[m1])
        k.op("act", lambda: nc.scalar.activation(out=ex.ap, in_=lg.ap, func=ACT.Exp, bias=m1.ap), outs=[ex], ins=[lg, m1])
        k.op("dve", lambda: nc.vector.tensor_tensor(out=ex.ap, in0=ex.ap, in1=sel.ap, op=ALU.mult), outs=[ex], ins=[ex, sel])
        k.op("dve", lambda: nc.vector.reduce_sum(out=sm.ap, in_=ex.ap, axis=mybir.AxisListType.X), outs=[sm], ins=[ex])
        k.op("dve", lambda: nc.vector.reciprocal(out=sm.ap, in_=sm.ap), outs=[sm], ins=[sm])
        k.op("dve", lambda: nc.vector.tensor_scalar(out=gates.ap[:, sub, :], in0=ex.ap, scalar1=sm.ap, scalar2=None, op0=ALU.mult),
             outs=[gates], ins=[ex, sm])
        if mstage == 4:
            continue
        pst = psum()
        k.pe([lambda: nc.tensor.transpose(out=pst.ap[0:NE, 0:P], in_=gates.ap[:, sub, :], identity=ident_f.ap)],
             outs=[pst], ins=[gates, ident_f])
        k.op("act", lambda: nc.scalar.copy(out=GT.ap[:, tcols], in_=pst.ap[0:NE, 0:P]), outs=[GT], ins=[pst])
    if mstage in (3, 4, 5, 31, 32, 33, 34):
        return dbg_exit()
    k.barrier()
    st_m1.close()
    cur["st"] = st
    HT = sb("HT", [P, KC, TOK], BF16)

    wstate["n"] = (wstate["n"] + NWB - 1) // NWB * NWB
    for e in range(n_exp):
        items = []
        for bi in range(8):
            holder = {}

            def fg_(buf, holder=holder):
                holder["g"] = buf

            def fu_(buf, bi=bi, holder=holder, e=e):
                gb = holder["g"]
                for jj in range(2):
                    j = bi * 2 + jj
                    bcol = e * KC + j
                    for tt in range(TOK // 512):
                        tc = slice(tt * 512, (tt + 1) * 512)
                        psg = psum()
                        k.mm(psg, [(gb[:, kk, jj * P:(jj + 1) * P], h2T[:, kk, tc]) for kk in range(KC)])
                        psu = psum()
                        k.mm(psu, [(buf[:, kk, jj * P:(jj + 1) * P], h2T[:, kk, tc]) for kk in range(KC)])
                        k.op("dve", lambda: nc.vector.tensor_scalar(out=mA.ap, in0=psg.ap, scalar1=ebg.ap[:, bcol:bcol + 1], scalar2=7.0,
                                                                   op0=ALU.add, op1=ALU.min), outs=[mA], ins=[psg, ebg])
                        k.op("act", lambda: nc.scalar.activation(out=mB.ap, in_=mA.ap, func=ACT.Sigmoid, scale=1.702), outs=[mB], ins=[mA])
                        k.op("dve", lambda: nc.vector.tensor_scalar(out=mC.ap, in0=psu.ap, scalar1=ebu.ap[:, bcol:bcol + 1], scalar2=7.0,
                                                                   op0=ALU.add, op1=ALU.min), outs=[mC], ins=[psu, ebu])
                        k.op("dve", lambda: nc.vector.tensor_scalar(out=mC.ap, in0=mC.ap, scalar1=-7.0, scalar2=1.0,
                                                                    op0=ALU.max, op1=ALU.add), outs=[mC], ins=[mC])
                        k.op("dve", lambda: nc.vector.tensor_tensor(out=mA.ap, in0=mA.ap, in1=mB.ap, op=ALU.mult), outs=[mA], ins=[mA, mB])
                        k.op("dve", lambda: nc.vector.tensor_tensor(out=HT.ap[:, j, tc], in0=mC.ap, in1=mA.ap, op=ALU.mult),
                             outs=[HT], ins=[mC, mA])
            items.append((ewg[e * 8 + bi], fg_))
            items.append((ewu[e * 8 + bi], fu_))
        for hb in range(4):
            holder = {}

            def fd0_(buf, holder=holder):
                holder["a"] = buf
                holder["slot"] = (wstate["n"] - 1)

            def fd1_(buf, hb=hb, e=e, holder=holder):
                ba = holder["a"]
                ia = [i for i in range(NWB) if wpool[i] is ba][0]
                ib = [i for i in range(NWB) if wpool[i] is buf][0]
                assert ib == ia + 1 and ia % 2 == 0, (ia, ib)
                cols = slice(hb * 512, (hb + 1) * 512)
                for sub in range(TOK // P):
                    psy = psum()
                    k.mm(psy, [(HT[:, kk, sub * P:(sub + 1) * P], V(wp_all[:][:, ia:ia + 2, kk, :], None)) for kk in range(KC)],
                         extra_ins=[ba, buf])
                    k.op("dve", lambda: nc.vector.scalar_tensor_tensor(out=mD.ap, in0=psy.ap,
                                                                      scalar=gates.ap[:, sub, e:e + 1], in1=gt2.ap[:, cols],
                                                                      op0=ALU.mult, op1=ALU.mult), outs=[mD], ins=[psy, gates, gt2])
                    k.op("dve", lambda: nc.vector.tensor_tensor(out=acc.ap[:, sub, cols], in0=acc.ap[:, sub, cols], in1=mD.ap,
                                                               op=ALU.add), outs=[acc], ins=[acc, mD])
            items.append((ewd[e * 8 + hb * 2], fd0_))
            items.append((ewd[e * 8 + hb * 2 + 1], fd1_))
        nxt_e = []
        if e + 1 < n_exp:
            for b in range(3):
                nxt_e += [ewg[(e + 1) * 8 + b], ewu[(e + 1) * 8 + b]]
        stream(items, nxt=nxt_e)
    if n_exp > 0:
        for bi in range(8):
            cols = slice(bi * WB, (bi + 1) * WB)
            for sub in range(TOK // P):
                psy = psum()
                k.mm(psy[:, 0:WB], [(GT[0:NE, sub * P:(sub + 1) * P], ebd[0:NE, cols])])
                k.op("dve", lambda: nc.vector.tensor_tensor(out=mD.ap[:, 0:WB], in0=psy.ap[:, 0:WB], in1=gt2.ap[:, cols], op=ALU.mult),
                     outs=[mD], ins=[psy, gt2])
                k.op("dve", lambda: nc.vector.tensor_tensor(out=acc.ap[:, sub, cols], in0=acc.ap[:, sub, cols], in1=mD.ap[:, 0:WB],
                                                            op=ALU.add), outs=[acc], ins=[acc, mD])
    fgb = gt2
    sp_load(fgb, fg_d)
    junk = sb_view(HT, F32, "p a b -> p (a b)")[:, 0:D]
    last = None
    for sub in range(TOK // P):
        ssq = small[:, 14:15]
        rs = small[:, 15:16]
        k.op("act", lambda: nc.scalar.activation(out=junk.ap, in_=acc.ap[:, sub, :], func=ACT.Square, accum_out=ssq.ap),
             outs=[junk, ssq], ins=[acc])
        rstd_from_ssq(rs, ssq, D)
        k.op("dve", lambda: nc.vector.scalar_tensor_tensor(out=acc.ap[:, sub, :], in0=acc.ap[:, sub, :], scalar=rs.ap, in1=fgb.ap,
                                                          op0=ALU.mult, op1=ALU.mult), outs=[acc], ins=[acc, rs, fgb])
        last = k.dma("sp", out_d[sub * P:(sub + 1) * P, :], acc[:, sub, :])
    k.wait_tok("sp", last)
    return nc, st


def _blk(w, ncols=WB):
    n = w.shape[1] // ncols
    return np.ascontiguousarray(w.reshape(KC, P, n, ncols).transpose(2, 1, 0, 3).reshape(n, P, KC * ncols))


def _bc(v, n=P):
    return np.ascontiguousarray(np.broadcast_to(np.asarray(v, np.float32).reshape(1, -1), (n, v.size)))


def _col(v):
    return np.ascontiguousarray(v.reshape(-1, P).T)


def _shared_inputs(inp, n_exp):
    f = np.float32
    w_in = np.asarray(inp["w_in"][0], f)
    ada_w = np.asarray(inp["ada_w"][0], f)
    ada_b = np.asarray(inp["ada_b"][0], f)
    w2aug = np.zeros((32, 1024), f)
    w2aug[0:16] = inp["gla_gate_w2"][0]
    w2aug[16] = inp["gla_gate_b"][0]
    s_idx = np.arange(P)[:, None]
    t_idx = np.arange(P)[None, :]
    sh = {
        "adaw": _blk(ada_w),
        "adabc": _col(ada_b[:4096]),
        "adabr": ada_b[4096:].reshape(1, 8192).copy(),
        "g1": _col(np.asarray(inp["norm1_g"][0], f)),
        "win": _blk(np.concatenate([w_in[:, :10240], w_in[:, 10256:]], axis=1)),
        "wglr": np.ascontiguousarray(w_in[:, 10240:10256].reshape(KC, P, 16).transpose(1, 0, 2).reshape(P, KC * 16)),
        "w2aug": w2aug,
        "lng": _bc(inp["sgu_ln_g"][0]),
        "lnb": _bc(inp["sgu_ln_b"][0]),
        "wmT": np.ascontiguousarray(np.asarray(inp["sgu_w"][0], f).transpose(2, 0, 1).reshape(P, 8 * P)),
        "sgub": np.asarray(inp["sgu_b"][0], f).reshape(1, 8 * P).copy(),
        "gn": _bc(inp["gla_norm_g"][0]),
        "wa": _blk(np.asarray(inp["w_branch_a"][0], f)),
        "wb": _blk(np.asarray(inp["w_branch_b"][0], f)),
        "wo": _blk(np.asarray(inp["w_out"][0], f)),
        "n2g": _bc(inp["norm2_g"][0]),
        "rw": np.ascontiguousarray(np.asarray(inp["router_w"][0], f).reshape(KC, P, NE).transpose(1, 0, 2).reshape(P, KC * NE)),
        "rb": _bc(inp["router_b"][0]),
        "fg": _bc(inp["final_g"]),
        "ident": np.eye(P, dtype=f),
        "tri": np.where(s_idx <= t_idx, -1.0 / 16.0, 0.0).astype(f),
        "trirev": np.where(s_idx > t_idx, -1.0 / 16.0, 0.0).astype(f),
        "cmask": np.where(s_idx <= t_idx, 1.0, 0.0).astype(f),
    }
    if n_exp > 0:
        sh["ewg"] = np.concatenate([_blk(np.asarray(inp["exp_w_gate"][0, e], f)) for e in range(n_exp)], axis=0)
        sh["ewu"] = np.concatenate([_blk(np.asarray(inp["exp_w_up"][0, e], f)) for e in range(n_exp)], axis=0)
        sh["ewd"] = np.concatenate([_blk(np.asarray(inp["exp_w_down"][0, e], f)) for e in range(n_exp)], axis=0)
        sh["ebg"] = np.ascontiguousarray(np.asarray(inp["exp_b_gate"][0], f).reshape(NE, KC, P).transpose(2, 0, 1).reshape(P, NE * KC))
        sh["ebu"] = np.ascontiguousarray(np.asarray(inp["exp_b_up"][0], f).reshape(NE, KC, P).transpose(2, 0, 1).reshape(P, NE * KC))
        sh["ebd"] = np.ascontiguousarray(np.asarray(inp["exp_b_down"][0], f))
    return sh


def _in_maps(inp, n_exp=NE):
    sh = _shared_inputs(inp, n_exp)
    x = np.asarray(inp["x"], np.float32)
    c = np.asarray(inp["c"], np.float32)
    maps = []
    for core in range(8):
        b, half = core // 2, core % 2
        m = dict(sh)
        m["xo"] = np.ascontiguousarray(x[b, half * TOK:(half + 1) * TOK])
        m["xp"] = np.ascontiguousarray(x[b, 0:TOK]) if half == 1 else np.zeros((TOK, D), np.float32)
        m["flag"] = np.full((P, 1), float(half), np.float32)
        m["cT"] = _col(c[b])
        maps.append(m)
    return maps


def kernel(**inputs):
    nc, st = _build(NE, None)
    maps = _in_maps(inputs, NE)
    res = run_bass_kernel_spmd(nc, maps, core_ids=list(range(8)))
    out = np.zeros((4, 2 * TOK, D), np.float32)
    for core in range(8):
        b, half = core // 2, core % 2
        out[b, half * TOK:(half + 1) * TOK] = np.asarray(res.results[core]["out"], np.float32)
    return out
```

```python
from contextlib import ExitStack

import numpy as np
import concourse.bass as bass
import concourse.mybir as mybir
from concourse.bass_utils import run_bass_kernel_spmd

ACT = mybir.ActivationFunctionType
ALU = mybir.AluOpType
F32 = mybir.dt.float32
BF16 = mybir.dt.bfloat16

P = 128
D = 2048
KC = 16
TOK = 1024
TT = 256
NSUB = TT // P
NTILE = TOK // TT
WB = 256
NWB = 6
NWB_X = 2
NE = 32
SEC_AU, SEC_AV, SEC_Q, SEC_K, SEC_V, SEC_R, SEC_GA, SEC_GB = 0, 8, 16, 20, 24, 32, 40, 48
EPS = 1e-6
GELU_A = 0.0713548162726
GELU_B = 1.5957691216057308


class Dep:
    __slots__ = ("w", "r")

    def __init__(self):
        self.w = None
        self.r = {}


class V:
    __slots__ = ("ap", "dep")

    def __init__(self, ap, dep=None):
        self.ap = ap
        self.dep = dep

    def __getitem__(self, idx):
        return V(self.ap[idx], self.dep)


class K:
    def __init__(self, nc, st):
        self.nc = nc
        self.st = st
        self.eng = {"pe": nc.tensor, "act": nc.scalar, "dve": nc.vector, "pool": nc.gpsimd, "sp": nc.sync}
        self.sem = {e: st.enter_context(nc.semaphore("sem_" + e)) for e in self.eng}
        self.cnt = {e: 0 for e in self.eng}
        self.waited = {e: {} for e in self.eng}
        self.dsem = {}
        self.dcnt = {}
        self.nps = 0

    def _sem_of(self, key):
        return self.sem[key] if key in self.sem else self.dsem[key]

    def _wait(self, e, tok):
        key, val = tok
        if self.waited[e].get(key, 0) >= val:
            return
        self.eng[e].wait_ge(self._sem_of(key), val)
        self.waited[e][key] = val

    def _pre(self, e, outs, ins):
        for v in ins:
            if v.dep is not None and v.dep.w is not None:
                self._wait(e, v.dep.w)
        for v in outs:
            if v.dep is None:
                continue
            if v.dep.w is not None:
                self._wait(e, v.dep.w)
            for key, val in v.dep.r.items():
                self._wait(e, (key, val))

    def _post(self, tok, outs, ins):
        for v in ins:
            if v.dep is not None:
                v.dep.r[tok[0]] = tok[1]
        for v in outs:
            if v.dep is not None:
                v.dep.w = tok
                v.dep.r = {}

    def op(self, e, build, outs=(), ins=()):
        self._pre(e, outs, ins)
        inst = build()
        self.cnt[e] += 1
        inst.then_inc(self.sem[e], 1)
        tok = (e, self.cnt[e])
        self._post(tok, outs, ins)
        return tok

    def pe(self, builds, outs, ins):
        self._pre("pe", outs, ins)
        inst = None
        for b in builds:
            inst = b()
        self.cnt["pe"] += 1
        inst.then_inc(self.sem["pe"], 1)
        tok = ("pe", self.cnt["pe"])
        self._post(tok, outs, ins)
        return tok

    def mm(self, out, pairs, extra_ins=()):
        n = len(pairs)
        nc = self.nc
        builds = [
            (lambda i=i, l=l, r=r: nc.tensor.matmul(out.ap, l.ap, r.ap, start=(i == 0), stop=(i == n - 1)))
            for i, (l, r) in enumerate(pairs)
        ]
        ins = [x for pr in pairs for x in pr] + list(extra_ins)
        return self.pe(builds, [out], ins)

    def dma(self, e, out, in_):
        self._pre(e, [out], [in_])
        d = out.dep if out.dep is not None else in_.dep
        key = ("d", id(d))
        if key not in self.dsem:
            self.dsem[key] = self.st.enter_context(self.nc.semaphore("dsem%d" % len(self.dsem)))
            self.dcnt[key] = 0
            self._keep = getattr(self, "_keep", [])
            self._keep.append(d)
        inst = self.eng[e].dma_start(out=out.ap, in_=in_.ap)
        self.dcnt[key] += 16
        inst.then_inc(self.dsem[key], 16)
        tok = (key, self.dcnt[key])
        self._post(tok, [out], [in_])
        return tok

    def wait_tok(self, e, tok):
        self._wait(e, tok)

    def barrier(self):
        toks = [(e, c) for e, c in self.cnt.items() if c > 0] + list(self.dcnt.items())
        for e in self.eng:
            for t in toks:
                if t[0] != e:
                    self._wait(e, t)


def _build(n_exp=NE, dbg=None, dbg_tiles=NTILE):
    nc = bass.Bass("TRN2", target_bir_lowering=False)
    st = ExitStack()
    k = K(nc, st)

    def din(name, shape, dt=F32):
        return V(nc.dram_tensor(name, list(shape), dt, kind="ExternalInput").ap(), None)

    xo = din("xo", [TOK, D])
    xp = din("xp", [TOK, D])
    flag_d = din("flag", [P, 1])
    cT_d = din("cT", [P, KC])
    adaw = din("adaw", [48, P, KC * WB])
    adabc_d = din("adabc", [P, 32])
    adabr_d = din("adabr", [1, 8192])
    g1_d = din("g1", [P, KC])
    win = din("win", [56, P, KC * WB])
    wglr_d = din("wglr", [P, KC * 16])
    w2aug_d = din("w2aug", [32, 1024])
    lng_d = din("lng", [P, D])
    lnb_d = din("lnb", [P, D])
    wmT_d = din("wmT", [P, 8 * P])
    sgub_d = din("sgub", [1, 8 * P])
    gn_d = din("gn", [P, 512])
    wa = din("wa", [8, P, KC * WB])
    wb = din("wb", [8, P, KC * WB])
    wo = din("wo", [8, P, KC * WB])
    n2g_d = din("n2g", [P, D])
    rw_d = din("rw", [P, KC * NE])
    rb_d = din("rb", [P, NE])
    fg_d = din("fg", [P, D])
    ident_d = din("ident", [P, P])
    tri_d = din("tri", [P, P])
    trirev_d = din("trirev", [P, P])
    cmask_d = din("cmask", [P, P])
    if n_exp > 0:
        ewg = din("ewg", [n_exp * 8, P, KC * WB])
        ewu = din("ewu", [n_exp * 8, P, KC * WB])
        ewd = din("ewd", [n_exp * 8, P, KC * WB])
        ebg_d = din("ebg", [P, NE * KC])
        ebu_d = din("ebu", [P, NE * KC])
        ebd_d = din("ebd", [NE, D])
    out_d = V(nc.dram_tensor("out", [TOK, D], F32, kind="ExternalOutput").ap(), Dep())
    x1_d = V(nc.dram_tensor("x1s", [TOK, D], F32, kind="Internal").ap(), Dep())
    dbg_d = None
    if dbg is not None:
        dbg_d = V(nc.dram_tensor("dbg", [TOK, D], F32, kind="ExternalOutput").ap(), Dep())

    cur = {"st": st}
    st_tm = ExitStack()

    def sb(name, shape, dt):
        t = cur["st"].enter_context(nc.sbuf_tensor("s_" + name, list(shape), dt))
        return V(t[:], Dep())

    def sbt(name, shape, dt):
        t = st_tm.enter_context(nc.sbuf_tensor("s_" + name, list(shape), dt))
        return V(t[:], Dep())

    def sb_view(v, dt, pattern=None, **kw):
        ap = v.ap if dt is None else v.ap.bitcast(dt)
        if pattern is not None:
            ap = ap.rearrange(pattern, **kw)
        return V(ap, v.dep)

    psf = []
    for i in range(7):
        t = st.enter_context(nc.psum_tensor("ps%d" % i, [P, 512], F32))
        psf.append(V(t[:], Dep()))
    tb = st.enter_context(nc.psum_tensor("psb", [P, 1024], BF16))
    psb = V(tb[:], Dep())

    def psum():
        v = psf[k.nps % 7]
        k.nps += 1
        return v

    ident_f = sb("ident_f", [P, P], F32)
    ident_b = sb("ident_b", [P, P], BF16)
    ones_row = sb("ones_row", [1, P], BF16)
    flag_sb = sb("flag_sb", [P, 1], F32)
    cT_sb = sb("cT_sb", [P, KC], F32)
    cond_f = sb("cond_f", [P, KC], F32)
    cond_b = sb("cond_b", [P, KC], BF16)
    adabc = sb("adabc", [P, 32], F32)
    adabr = sb("adabr", [1, WB], BF16)
    g1_sb = sb("g1_sb", [P, KC], F32)
    modc = sb("modc", [P, 32], F32)
    scale1 = sb("scale1", [P, KC], F32)
    small = sb("small", [P, 64], F32)
    wp_all = st.enter_context(nc.sbuf_tensor("s_wp_all", [P, NWB, KC, WB], BF16))
    wpool = [V(wp_all[:][:, i], Dep()) for i in range(NWB)]
    wstate = {"n": 0}
    wp_x = st_tm.enter_context(nc.sbuf_tensor("s_wp_x", [P, NWB_X, KC, WB], BF16))
    wpool += [V(wp_x[:][:, i], Dep()) for i in range(NWB_X)]
    tri_f = sbt("tri_f", [P, P], F32)
    trirev_f = sbt("trirev_f", [P, P], F32)
    cmask_f = sbt("cmask_f", [P, P], F32)
    wglr = sbt("wglr", [P, KC, 16], BF16)
    w2aug = sbt("w2aug", [32, 1024], F32)
    wmT = sbt("wmT", [P, 8, P], BF16)
    sgub = sbt("sgub", [1, 8 * P], BF16)
    gn_bc = sbt("gn_bc", [P, 512], F32)
    bcA = sbt("bcA", [P, D], F32)

    def sp_load(dst, src):
        return k.dma("sp", dst, src)

    sp_load(ident_f, ident_d)
    sp_load(tri_f, tri_d)
    sp_load(trirev_f, trirev_d)
    sp_load(cmask_f, cmask_d)
    sp_load(flag_sb, flag_d)
    sp_load(cT_sb, cT_d)
    sp_load(adabc, adabc_d)
    sp_load(g1_sb, g1_d)
    sp_load(w2aug, w2aug_d)
    sp_load(gn_bc, gn_d)
    k.dma("pool", sb_view(wglr, None, "p a b -> p (a b)"), wglr_d)
    k.dma("pool", sb_view(wmT, None, "p a b -> p (a b)"), wmT_d)
    k.dma("pool", sgub, sgub_d)
    k.op("dve", lambda: nc.vector.memset(ones_row.ap, 1.0), outs=[ones_row])
    k.op("dve", lambda: nc.vector.tensor_copy(out=ident_b.ap, in_=ident_f.ap), outs=[ident_b], ins=[ident_f])
    k.op("dve", lambda: nc.vector.memset(wmT.ap[64:128, :, 0:64], 0.0), outs=[wmT])

    def skey(v):
        return str(v.ap)

    wc_d = V(nc.dram_tensor("wcache", [80, P, KC * WB], BF16, kind="Internal").ap(), Dep())
    wcache = {}

    def cacheable(src):
        s = skey(src)
        return any(("'%s'" % nm) in s for nm in ("win", "wa", "wb", "wo"))

    def wload(src):
        buf = wpool[wstate["n"] % len(wpool)]
        wstate["n"] += 1
        flat = sb_view(buf, None, "p a b -> p (a b)")
        key = skey(src)
        if key in wcache and wcache[key][1]:
            k.dma("sp", flat, wc_d[wcache[key][0]])
        else:
            k.dma("pool", flat, src)
        return buf

    def wstore(src, buf):
        if not cacheable(src):
            return
        key = skey(src)
        if key in wcache:
            return
        idx = len(wcache)
        wcache[key] = [idx, False]
        k.dma("sp", wc_d[idx], sb_view(buf, None, "p a b -> p (a b)"))
        wcache[key][1] = True

    pf = []

    def stream(items, nxt=(), depth=None):
        depth = len(wpool) - 1 if depth is None else depth
        srcs = [it[0] for it in items] + list(nxt)[:depth]
        n = len(items)
        loaded = []
        for key, buf in pf:
            assert key == skey(srcs[len(loaded)]), "prefetch order mismatch"
            loaded.append(buf)
        del pf[:]

        def ensure(i):
            while len(loaded) <= i:
                loaded.append(wload(srcs[len(loaded)]))

        for i in range(min(depth, len(srcs))):
            ensure(i)
        for i in range(n):
            ensure(i)
            wstore(srcs[i], loaded[i])
            items[i][1](loaded[i])
            if i + depth < len(srcs):
                ensure(i + depth)
        for i in range(n, len(loaded)):
            pf.append((skey(srcs[i]), loaded[i]))

    k.op("act", lambda: nc.scalar.activation(out=cond_f.ap, in_=cT_sb.ap, func=ACT.Silu), outs=[cond_f], ins=[cT_sb])
    k.op("dve", lambda: nc.vector.tensor_copy(out=cond_b.ap, in_=cond_f.ap), outs=[cond_b], ins=[cond_f])
    cr = {}

    def build_condrep(tag):
        ones_f = sb("ones_f" + tag, [P, P], F32)
        crep = sb("condrep" + tag, [P, KC, P], BF16)
        k.op("dve", lambda: nc.vector.memset(ones_f.ap, 1.0), outs=[ones_f])
        for kk in range(KC):
            k.op("dve", lambda kk=kk: nc.vector.tensor_scalar(out=crep.ap[:, kk, :], in0=ones_f.ap,
                                                             scalar1=cond_f.ap[:, kk:kk + 1], scalar2=None, op0=ALU.mult),
                 outs=[crep], ins=[ones_f, cond_f])
        cr["v"] = crep

    psA = psum()

    def modcol_item(bi):
        def fn(buf):
            for jj in range(2):
                j = bi * 2 + jj
                k.mm(psA[:, j:j + 1], [(buf[:, kk, jj * P:(jj + 1) * P], cond_b[:, kk:kk + 1]) for kk in range(KC)])
        return (adaw[bi], fn)

    def modrow_seg(seg, dst, post=None):
        items = []
        for bb in range(8):
            bi = seg * 8 + bb

            def fn(buf, bb=bb):
                ps = psum()
                c0 = (seg - 2) * D + bb * WB
                k.dma("pool", adabr, adabr_d[:, c0:c0 + WB])
                pairs = [(cr["v"][:, kk, :], buf[:, kk, :]) for kk in range(KC)]
                pairs.append((ones_row[0:1, :], adabr[0:1, :]))
                k.mm(ps[:, 0:WB], pairs)
                k.op("act", lambda: nc.scalar.copy(out=dst.ap[:, bb * WB:(bb + 1) * WB], in_=ps.ap[:, 0:WB]),
                     outs=[dst], ins=[ps])
            items.append((adaw[bi], fn))
        return items

    stream([modcol_item(bi) for bi in range(16)])
    k.op("dve", lambda: nc.vector.tensor_tensor(out=modc.ap, in0=psA.ap[:, 0:32], in1=adabc.ap, op=ALU.add),
         outs=[modc], ins=[psA, adabc])
    shift1 = modc
    k.op("dve", lambda: nc.vector.scalar_tensor_tensor(out=scale1.ap, in0=modc.ap[:, 16:32], scalar=1.0, in1=g1_sb.ap,
                                                      op0=ALU.add, op1=ALU.mult),
         outs=[scale1], ins=[modc, g1_sb])
    cur["st"] = st_tm
    build_condrep("_tm")
    cur["st"] = st
    gt1_items = modrow_seg(2, bcA)

    cur["st"] = st_tm
    xs = sb("xs", [P, D], F32)
    hT = sb("hT", [P, KC, TT], BF16)
    lbuf = sb("lbuf", [P, NSUB, 1024], F32)
    ks = sb("ks", [P, NSUB, 1024], BF16)
    vtok = sb("vtok", [P, NSUB, D], BF16)
    qdT = sb("qdT", [P, 8, TT], BF16)
    kiT = sb("kiT", [P, 8, TT], BF16)
    sr = sb("sr", [P, NSUB, D], BF16)
    yb = sb("yb", [P, D], BF16)
    ybT = sb("ybT", [P, KC, TT], BF16)
    glrT = sb("glrT", [32, TT], F32)
    dT = sb("dT", [P, 8, NSUB], F32)
    S32 = sb("S32", [P, 8, 512], F32)
    Sbf = sb("Sbf", [P, 8, 512], BF16)
    t_all = cur["st"].enter_context(nc.sbuf_tensor("s_t_all", [P, 8, 256], F32))
    tpool = [V(t_all[:][:, i, :], Dep()) for i in range(8)]
    tstate = {"n": 0}

    def tmp():
        v = tpool[tstate["n"] % 8]
        tstate["n"] += 1
        return v

    attm2 = [sb("attm%d" % i, [P, P], BF16) for i in range(2)]
    lnbc = sb("lnbc", [P, 2, D], BF16)
    stat = sb("stat", [P, 64], F32)
    guT = sb_view(xs, BF16, "p (a b) -> p a b", a=KC)[:, :, 0:TT]
    gv = sb("gv", [P, NSUB, D], BF16)
    sgaT = sb_view(vtok, None, "p s (a b) -> p (s a) b", b=TT)
    sgbT = sb_view(sr, None, "p s (a b) -> p (s a) b", b=TT)

    k.dma("pool", lnbc[:, 0, :], lng_d)
    k.dma("pool", lnbc[:, 1, :], lnb_d)
    k.op("dve", lambda: nc.vector.memset(glrT.ap, 1.0), outs=[glrT])
    k.op("dve", lambda: nc.vector.memset(S32.ap, 0.0), outs=[S32])
    k.op("dve", lambda: nc.vector.memset(Sbf.ap, 0.0), outs=[Sbf])

    def rstd_from_ssq(dst, ssq, n):
        k.op("dve", lambda: nc.vector.tensor_scalar(out=dst.ap, in0=ssq.ap, scalar1=1.0 / n, scalar2=EPS,
                                                   op0=ALU.mult, op1=ALU.add), outs=[dst], ins=[ssq])
        k.op("act", lambda: nc.scalar.activation(out=dst.ap, in_=dst.ap, func=ACT.Ln), outs=[dst], ins=[dst])
        k.op("act", lambda: nc.scalar.activation(out=dst.ap, in_=dst.ap, func=ACT.Exp, scale=-0.5), outs=[dst], ins=[dst])

    def gelu(ps_v, out_v, n, accum=None):
        a = tmp()[:, 0:n]
        b = tmp()[:, 0:n]
        k.op("act", lambda: nc.scalar.activation(out=a.ap, in_=ps_v.ap, func=ACT.Square), outs=[a], ins=[ps_v])
        k.op("dve", lambda: nc.vector.tensor_scalar(out=a.ap, in0=a.ap, scalar1=GELU_A, scalar2=GELU_B,
                                                   op0=ALU.mult, op1=ALU.add), outs=[a], ins=[a])
        k.op("dve", lambda: nc.vector.tensor_tensor(out=a.ap, in0=a.ap, in1=ps_v.ap, op=ALU.mult), outs=[a], ins=[a, ps_v])
        k.op("act", lambda: nc.scalar.activation(out=b.ap, in_=a.ap, func=ACT.Sigmoid), outs=[b], ins=[a])
        if accum is None:
            k.op("dve", lambda: nc.vector.tensor_tensor(out=out_v.ap, in0=b.ap, in1=ps_v.ap, op=ALU.mult),
                 outs=[out_v], ins=[b, ps_v])
        else:
            k.op("dve", lambda: nc.vector.scalar_tensor_tensor(out=out_v.ap, in0=b.ap, scalar=1.0, in1=ps_v.ap,
                                                              op0=ALU.mult, op1=ALU.mult, accum_out=accum.ap),
                 outs=[out_v, accum], ins=[b, ps_v])

    def srcs_s1(prefix):
        if prefix:
            return [win[SEC_K + b] for b in range(4)] + [win[SEC_V + b] for b in range(8)]
        out = [win[SEC_V + b] for b in range(8)]
        for b in range(4):
            out += [win[SEC_Q + b], win[SEC_K + b]]
        return out + [win[SEC_R + b] for b in range(8)]

    def srcs_s2():
        return [win[SEC_AU + b] for b in range(8)] + [win[SEC_AV + b] for b in range(8)]

    def srcs_s3():
        return [win[SEC_GA + b] for b in range(8)] + [win[SEC_GB + b] for b in range(8)] + [wa[b] for b in range(8)]

    def prep(xsrc, t0):
        for s in range(NSUB):
            sp_load(xs, xsrc[t0 + s * P:t0 + (s + 1) * P, :])
            ssq = small[:, 0:1]
            rs = small[:, 1:2]
            k.op("act", lambda: nc.scalar.activation(out=yb.ap, in_=xs.ap, func=ACT.Square, accum_out=ssq.ap),
                 outs=[yb, ssq], ins=[xs])
            rstd_from_ssq(rs, ssq, D)
            k.op("dve", lambda: nc.vector.tensor_scalar(out=xs.ap, in0=xs.ap, scalar1=rs.ap, scalar2=None,
                                                       op0=ALU.mult), outs=[xs], ins=[xs, rs])
            yield
            for kq in range(4):
                ps = psum()
                k.pe([(lambda kk=kk: nc.tensor.transpose(out=ps.ap[:, kk * P:(kk + 1) * P],
                                                        in_=xs.ap[:, (kq * 4 + kk) * P:(kq * 4 + kk + 1) * P],
                                                        identity=ident_f.ap)) for kk in range(4)],
                     outs=[ps], ins=[xs, ident_f])
                for kk in range(4):
                    kc = kq * 4 + kk
                    if kk % 2 == 0:
                        k.op("act", lambda kk=kk, kc=kc: nc.scalar.activation(
                            out=hT.ap[:, kc, s * P:(s + 1) * P], in_=ps.ap[:, kk * P:(kk + 1) * P], func=ACT.Identity,
                            scale=scale1.ap[:, kc:kc + 1], bias=shift1.ap[:, kc:kc + 1]),
                            outs=[hT], ins=[ps, scale1, shift1])
                    else:
                        k.op("dve", lambda kk=kk, kc=kc: nc.vector.tensor_scalar(
                            out=hT.ap[:, kc, s * P:(s + 1) * P], in0=ps.ap[:, kk * P:(kk + 1) * P],
                            scalar1=scale1.ap[:, kc:kc + 1], scalar2=shift1.ap[:, kc:kc + 1], op0=ALU.mult, op1=ALU.add),
                            outs=[hT], ins=[ps, scale1, shift1])

        yield
        ps = psum()
        k.mm(ps[0:16, 0:TT], [(wglr[:, kk, :], hT[:, kk, :]) for kk in range(KC)])
        k.op("act", lambda: nc.scalar.copy(out=glrT.ap[0:16, :], in_=ps.ap[0:16, 0:TT]), outs=[glrT], ins=[ps])
        for s in range(NSUB):
            for nb in range(4):
                ps = psum()
                k.mm(ps[:, 0:WB], [(glrT[0:32, s * P:(s + 1) * P], w2aug[0:32, nb * WB:(nb + 1) * WB])])
                tz = tmp()
                k.op("act", lambda: nc.scalar.activation(out=tz.ap, in_=ps.ap[:, 0:WB], func=ACT.Exp, scale=-1.0),
                     outs=[tz], ins=[ps])
                k.op("act", lambda: nc.scalar.activation(out=lbuf.ap[:, s, nb * WB:(nb + 1) * WB], in_=tz.ap,
                                                        func=ACT.Ln, bias=1.0), outs=[lbuf], ins=[tz])


    def token_tile(xsrc, t0, prefix, nxt_tile=(), do_prep=True, next_prep=None, extra_items=()):
        if do_prep:
            for _ in prep(xsrc, t0):
                pass

        items = []

        def ks_block(buf, bi):
            if True:
                for s in range(NSUB):
                    cols = slice(bi * WB, (bi + 1) * WB)
                    psr = psum()
                    k.mm(psr[:, 0:WB], [(trirev_f, lbuf[:, s, cols])])
                    ter = tmp()
                    k.op("act", lambda: nc.scalar.activation(out=ter.ap, in_=psr.ap[:, 0:WB], func=ACT.Exp),
                         outs=[ter], ins=[psr])
                    psk = psum()
                    k.mm(psk[:, 0:WB], [(hT[:, kk, s * P:(s + 1) * P], buf[:, kk, :]) for kk in range(KC)])
                    k.op("dve", lambda: nc.vector.tensor_tensor(out=ks.ap[:, s, cols], in0=psk.ap[:, 0:WB],
                                                               in1=ter.ap, op=ALU.mult),
                         outs=[ks], ins=[psk, ter])

        if prefix:
            for bi in range(4):
                items.append((win[SEC_K + bi], (lambda buf, bi=bi: ks_block(buf, bi))))
        for bi in range(8):
            def fn(buf, bi=bi):
                for s in range(NSUB):
                    psv = psum()
                    k.mm(psv[:, 0:WB], [(hT[:, kk, s * P:(s + 1) * P], buf[:, kk, :]) for kk in range(KC)])
                    k.op("act", lambda: nc.scalar.copy(out=vtok.ap[:, s, bi * WB:(bi + 1) * WB], in_=psv.ap[:, 0:WB]),
                         outs=[vtok], ins=[psv])
            items.append((win[SEC_V + bi], fn))

        def bT_chunk(j, need_exp):
            pst = psum()
            for s in range(NSUB):
                k.mm(pst[:, s * P:(s + 1) * P], [(lbuf[:, s, j * P:(j + 1) * P], tri_f)])
            for s in range(NSUB):
                k.op("act", lambda s=s: nc.scalar.activation(out=dT.ap[:, j, s:s + 1],
                                                            in_=pst.ap[:, s * P + P - 1:s * P + P], func=ACT.Exp),
                     outs=[dT], ins=[pst])
            if need_exp:
                teb = tmp()
                ten = tmp()
                k.op("act", lambda: nc.scalar.activation(out=teb.ap, in_=pst.ap[:, 0:TT], func=ACT.Exp),
                     outs=[teb], ins=[pst])
                k.op("act", lambda: nc.scalar.activation(out=ten.ap, in_=pst.ap[:, 0:TT], func=ACT.Exp, scale=-1.0),
                     outs=[ten], ins=[pst])
                return teb, ten
            return None, None

        if prefix:
            for j in range(8):
                bT_chunk(j, False)
        else:
            for bi in range(4):
                holder = {}

                def fq(buf, bi=bi, holder=holder):
                    holder["q"] = buf

                def fk(buf, bi=bi, holder=holder):
                    qb = holder["q"]
                    for jj in range(2):
                        j = bi * 2 + jj
                        teb, ten = bT_chunk(j, True)
                        psq = psum()
                        k.mm(psq[:, 0:TT], [(qb[:, kk, jj * P:(jj + 1) * P], hT[:, kk, :]) for kk in range(KC)])
                        k.op("dve", lambda: nc.vector.scalar_tensor_tensor(out=qdT.ap[:, j, :], in0=psq.ap[:, 0:TT], scalar=0.0625,
                                                                          in1=teb.ap, op0=ALU.mult, op1=ALU.mult),
                             outs=[qdT], ins=[psq, teb])
                        psk = psum()
                        k.mm(psk[:, 0:TT], [(buf[:, kk, jj * P:(jj + 1) * P], hT[:, kk, :]) for kk in range(KC)])
                        k.op("dve", lambda: nc.vector.tensor_tensor(out=kiT.ap[:, j, :], in0=psk.ap[:, 0:TT], in1=ten.ap,
                                                                   op=ALU.mult), outs=[kiT], ins=[psk, ten])
                    ks_block(buf, bi)
                items.append((win[SEC_Q + bi], fq))
                items.append((win[SEC_K + bi], fk))
            for bi in range(8):
                def fn(buf, bi=bi):
                    for s in range(NSUB):
                        psr = psum()
                        k.mm(psr[:, 0:WB], [(hT[:, kk, s * P:(s + 1) * P], buf[:, kk, :]) for kk in range(KC)])
                        tsr = tmp()
                        k.op("act", lambda: nc.scalar.activation(out=tsr.ap, in_=psr.ap[:, 0:WB], func=ACT.Silu),
                             outs=[tsr], ins=[psr])
                        gc = (bi * WB) % 512
                        k.op("dve", lambda: nc.vector.tensor_tensor(out=sr.ap[:, s, bi * WB:(bi + 1) * WB], in0=tsr.ap,
                                                                   in1=gn_bc.ap[:, gc:gc + WB], op=ALU.mult),
                             outs=[sr], ins=[tsr, gn_bc])
                items.append((win[SEC_R + bi], fn))
        items += list(extra_items)
        stream(items, nxt=(nxt_tile if prefix else srcs_s2()))
        if prefix and next_prep is not None:
            next(next_prep, None)

        for s in range(NSUB):
            tcols = slice(s * P, (s + 1) * P)
            if not prefix:
                for hd in range(4):
                    psa = psum()
                    k.mm(psa[:, 0:P], [(kiT[:, hd * 2 + kc, tcols], qdT[:, hd * 2 + kc, tcols]) for kc in range(2)])
                    attm = attm2[hd % 2]
                    k.op("dve", lambda: nc.vector.tensor_tensor(out=attm.ap, in0=psa.ap[:, 0:P], in1=cmask_f.ap, op=ALU.mult),
                         outs=[attm], ins=[psa, cmask_f])
                    pso = psum()
                    k.mm(pso[:, 0:512], [(attm, vtok[:, s, hd * 512:(hd + 1) * 512]),
                                         (qdT[:, hd * 2, tcols], Sbf[:, hd * 2, :]),
                                         (qdT[:, hd * 2 + 1, tcols], Sbf[:, hd * 2 + 1, :])])
                    ssq = small[:, 16 + 2 * hd:17 + 2 * hd]
                    rs = small[:, 17 + 2 * hd:18 + 2 * hd]
                    k.op("act", lambda hd=hd: nc.scalar.activation(out=yb.ap[:, hd * 512:(hd + 1) * 512], in_=pso.ap, func=ACT.Square,
                                                                  accum_out=ssq.ap), outs=[yb, ssq], ins=[pso])
                    rstd_from_ssq(rs, ssq, 512)
                    k.op("dve", lambda hd=hd: nc.vector.scalar_tensor_tensor(
                        out=yb.ap[:, hd * 512:(hd + 1) * 512], in0=pso.ap, scalar=rs.ap, in1=sr.ap[:, s, hd * 512:(hd + 1) * 512],
                        op0=ALU.mult, op1=ALU.mult), outs=[yb], ins=[pso, rs, sr])
                for half in range(2):
                    k.pe([(lambda kk=kk: nc.tensor.transpose(out=psb.ap[:, kk * P:(kk + 1) * P],
                                                            in_=yb.ap[:, (half * 8 + kk) * P:(half * 8 + kk + 1) * P],
                                                            identity=ident_b.ap)) for kk in range(8)],
                         outs=[psb], ins=[yb, ident_b])
                    k.op("act", lambda half=half: nc.scalar.copy(
                        out=ybT.ap[:, half * 8:(half + 1) * 8, tcols],
                        in_=psb.ap.rearrange("p (a b) -> p a b", a=8)), outs=[ybT], ins=[psb])
            for hd in range(4):
                if prefix and next_prep is not None and hd in (0, 2) and not (s == 0 and hd == 0):
                    next(next_prep, None)
                for kc in range(2):
                    idx = hd * 2 + kc
                    pss = psum()
                    k.mm(pss[:, 0:512], [(ks[:, s, idx * P:(idx + 1) * P], vtok[:, s, hd * 512:(hd + 1) * 512])])
                    k.op("dve", lambda idx=idx: nc.vector.scalar_tensor_tensor(
                        out=S32.ap[:, idx, :], in0=S32.ap[:, idx, :], scalar=dT.ap[:, idx, s:s + 1], in1=pss.ap,
                        op0=ALU.mult, op1=ALU.add), outs=[S32], ins=[S32, dT, pss])
                    k.op("act", lambda idx=idx: nc.scalar.copy(out=Sbf.ap[:, idx, :], in_=S32.ap[:, idx, :]),
                         outs=[Sbf], ins=[S32])
        if prefix:
            if next_prep is not None:
                for _ in next_prep:
                    pass
            return

        items = []
        for bi in range(8):
            def fn(buf, bi=bi):
                for jj in range(2):
                    j = bi * 2 + jj
                    psu = psum()
                    k.mm(psu[:, 0:TT], [(buf[:, kk, jj * P:(jj + 1) * P], hT[:, kk, :]) for kk in range(KC)])
                    gelu(psu[:, 0:TT], guT[:, j, :], TT)
            items.append((win[SEC_AU + bi], fn))
        for bi in range(8):
            def fn(buf, bi=bi):
                for s in range(NSUB):
                    psv = psum()
                    k.mm(psv[:, 0:WB], [(hT[:, kk, s * P:(s + 1) * P], buf[:, kk, :]) for kk in range(KC)])
                    gelu(psv[:, 0:WB], gv[:, s, bi * WB:(bi + 1) * WB], WB, accum=stat[:, s * 16 + bi:s * 16 + bi + 1])
                    tj = tmp()
                    k.op("act", lambda: nc.scalar.activation(out=tj.ap, in_=gv.ap[:, s, bi * WB:(bi + 1) * WB],
                                                            func=ACT.Square, accum_out=stat.ap[:, s * 16 + 8 + bi:s * 16 + 9 + bi]),
                         outs=[tj, stat], ins=[gv])
            items.append((win[SEC_AV + bi], fn))
        stream(items, nxt=srcs_s3())
        for s in range(NSUB):
            mean = small[:, 4:5]
            ex2 = small[:, 5:6]
            var = small[:, 6:7]
            rs = small[:, 7:8]
            k.op("dve", lambda: nc.vector.reduce_sum(out=mean.ap, in_=stat.ap[:, s * 16:s * 16 + 8], axis=mybir.AxisListType.X),
                 outs=[mean], ins=[stat])
            k.op("dve", lambda: nc.vector.reduce_sum(out=ex2.ap, in_=stat.ap[:, s * 16 + 8:s * 16 + 16], axis=mybir.AxisListType.X),
                 outs=[ex2], ins=[stat])
            k.op("dve", lambda: nc.vector.tensor_scalar(out=mean.ap, in0=mean.ap, scalar1=1.0 / D, scalar2=None, op0=ALU.mult),
                 outs=[mean], ins=[mean])
            k.op("dve", lambda: nc.vector.tensor_tensor(out=var.ap, in0=mean.ap, in1=mean.ap, op=ALU.mult), outs=[var], ins=[mean])
            k.op("dve", lambda: nc.vector.scalar_tensor_tensor(out=var.ap, in0=ex2.ap, scalar=1.0 / D, in1=var.ap,
                                                              op0=ALU.mult, op1=ALU.subtract), outs=[var], ins=[ex2, var])
            rstd_from_ssq(rs, var, 1.0)
            k.op("dve", lambda: nc.vector.tensor_scalar(out=gv.ap[:, s, :], in0=gv.ap[:, s, :], scalar1=mean.ap, scalar2=rs.ap,
                                                       op0=ALU.subtract, op1=ALU.mult), outs=[gv], ins=[gv, mean, rs])
            k.op("dve", lambda: nc.vector.tensor_tensor(out=gv.ap[:, s, :], in0=gv.ap[:, s, :], in1=lnbc.ap[:, 0, :], op=ALU.mult),
                 outs=[gv], ins=[gv, lnbc])
            k.op("dve", lambda: nc.vector.tensor_tensor(out=gv.ap[:, s, :], in0=gv.ap[:, s, :], in1=lnbc.ap[:, 1, :], op=ALU.add),
                 outs=[gv], ins=[gv, lnbc])
            for jq in range(4):
                psm = psum()
                for jj in range(4):
                    j = jq * 4 + jj
                    g = j // 2
                    k.mm(psm[:, jj * P:(jj + 1) * P], [(gv[:, s, j * P:(j + 1) * P], wmT[:, g, :]),
                                                        (ones_row[0:1, :], sgub[0:1, g * P:(g + 1) * P])])
                k.op("dve", lambda jq=jq: nc.vector.tensor_tensor(
                    out=guT.ap[:, jq * 4:(jq + 1) * 4, s * P:(s + 1) * P], in0=guT.ap[:, jq * 4:(jq + 1) * 4, s * P:(s + 1) * P],
                    in1=psm.ap.rearrange("p (a b) -> p a b", a=4), op=ALU.mult), outs=[guT], ins=[guT, psm])
        yaT = guT

        items = []
        for bi in range(8):
            def fn(buf, bi=bi):
                for jj in range(2):
                    m = bi * 2 + jj
                    psg = psum()
                    k.mm(psg[:, 0:TT], [(buf[:, kk, jj * P:(jj + 1) * P], hT[:, kk, :]) for kk in range(KC)])
                    k.op("act", lambda: nc.scalar.activation(out=sgaT.ap[:, m, :], in_=psg.ap[:, 0:TT], func=ACT.Sigmoid),
                         outs=[sgaT], ins=[psg])
            items.append((win[SEC_GA + bi], fn))
        for bi in range(8):
            def fn(buf, bi=bi):
                for jj in range(2):
                    m = bi * 2 + jj
                    psg = psum()
                    k.mm(psg[:, 0:TT], [(buf[:, kk, jj * P:(jj + 1) * P], hT[:, kk, :]) for kk in range(KC)])
                    k.op("act", lambda: nc.scalar.activation(out=sgbT.ap[:, m, :], in_=psg.ap[:, 0:TT], func=ACT.Sigmoid),
                         outs=[sgbT], ins=[psg])
            items.append((win[SEC_GB + bi], fn))
        for bi in range(8):
            def fn(buf, bi=bi):
                for jj in range(2):
                    m = bi * 2 + jj
                    psp = psum()
                    k.mm(psp[:, 0:TT], [(buf[:, kk, jj * P:(jj + 1) * P], yaT[:, kk, :]) for kk in range(KC)])
                    k.op("dve", lambda: nc.vector.tensor_tensor(out=sgaT.ap[:, m, :], in0=psp.ap[:, 0:TT], in1=sgaT.ap[:, m, :],
                                                               op=ALU.mult), outs=[sgaT], ins=[psp, sgaT])
            items.append((wa[bi], fn))
        for bi in range(8):
            def fn(buf, bi=bi):
                if bi in (0, 3, 6) and next_prep is not None:
                    next(next_prep, None)
                for jj in range(2):
                    m = bi * 2 + jj
                    psp = psum()
                    k.mm(psp[:, 0:TT], [(buf[:, kk, jj * P:(jj + 1) * P], ybT[:, kk, :]) for kk in range(KC)])
                    k.op("dve", lambda: nc.vector.tensor_tensor(out=sgbT.ap[:, m, :], in0=psp.ap[:, 0:TT], in1=sgbT.ap[:, m, :],
                                                               op=ALU.mult), outs=[sgbT], ins=[psp, sgbT])
                    k.op("dve", lambda: nc.vector.tensor_tensor(out=sgbT.ap[:, m, :], in0=sgbT.ap[:, m, :], in1=sgaT.ap[:, m, :],
                                                                op=ALU.add), outs=[sgbT], ins=[sgbT, sgaT])
            items.append((wb[bi], fn))
        mT = sgbT
        for bi in range(8):
            def fn(buf, bi=bi):
                if bi == 1 and next_prep is not None:
                    for _ in next_prep:
                        pass
                cols = slice(bi * WB, (bi + 1) * WB)
                xpcs = []
                for s in range(NSUB):
                    xpc = tmp()
                    sp_load(xpc, xsrc[t0 + s * P:t0 + (s + 1) * P, cols])
                    xpcs.append(xpc)
                for s in range(NSUB):
                    pso = psum()
                    k.mm(pso[:, 0:WB], [(mT[:, kk, s * P:(s + 1) * P], buf[:, kk, :]) for kk in range(KC)])
                    rows = slice(t0 + s * P, t0 + (s + 1) * P)
                    xpc = xpcs[s]
                    to = tmp()
                    k.op("dve", lambda: nc.vector.tensor_tensor(out=to.ap, in0=pso.ap[:, 0:WB], in1=bcA.ap[:, cols],
                                                               op=ALU.mult), outs=[to], ins=[pso, bcA])
                    k.op("dve", lambda: nc.vector.tensor_tensor(out=xpc.ap, in0=xpc.ap, in1=to.ap, op=ALU.add),
                         outs=[xpc], ins=[xpc, to])
                    sp_load(x1_d[rows, cols], xpc)
            items.append((wo[bi], fn))
        stream(items, nxt=nxt_tile)


    n_pref = NTILE if dbg != "noprefix" else 0
    mstage = int(dbg[1:]) if (dbg is not None and dbg.startswith("m")) else 99
    n_main = NTILE if (dbg is None or mstage != 99) else int(dbg_tiles)
    tiles = [(xp, ti * TT, True) for ti in range(n_pref)] + [(xo, ti * TT, False) for ti in range(n_main)]
    for i, (xsrc_, t0_, pre_) in enumerate(tiles):
        nx = tiles[i + 1] if i + 1 < len(tiles) else None
        if i == n_pref and n_pref > 0:
            pass
        token_tile(xsrc_, t0_, pre_,
                   nxt_tile=(srcs_s1(nx[2]) if nx is not None else ()),
                   do_prep=(i == 0),
                   extra_items=(gt1_items if i == 0 else ()),
                   next_prep=(prep(nx[0], nx[1]) if nx is not None else None))
        if pre_ and (nx is None or not nx[2]):
            k.op("dve", lambda: nc.vector.tensor_scalar(out=S32.ap, in0=S32.ap, scalar1=flag_sb.ap, scalar2=None, op0=ALU.mult),
                 outs=[S32], ins=[S32, flag_sb])
            k.op("act", lambda: nc.scalar.copy(out=Sbf.ap, in_=S32.ap), outs=[Sbf], ins=[S32])
    k.barrier()
    st_tm.close()
    cur["st"] = st
    del wpool[NWB:]
    wstate["n"] = 0

    def dbg_exit():
        tok = k.dma("sp", dbg_d, x1_d)
        k.wait_tok("sp", tok)
        return nc, st

    if dbg is not None and mstage == 99:
        return dbg_exit()

    acc = sb("acc", [P, TOK // P, D], F32)
    h2T = sb("h2T", [P, KC, TOK], BF16)
    gt2 = sb("gt2", [P, D], F32)
    gates = sb("gates", [P, TOK // P, NE], F32)
    GT = sb("GT", [NE, TOK], BF16)
    ebg = sb("ebg", [P, NE * KC], F32)
    ebu = sb("ebu", [P, NE * KC], F32)
    ebd = sb("ebd", [NE, D], BF16)
    rw = sb("rw", [P, KC, NE], F32)
    rb = sb("rb", [P, NE], F32)
    m_all = st.enter_context(nc.sbuf_tensor("s_m_all", [P, 4, 512], F32))
    mA, mB, mC, mD = [V(m_all[:][:, i, :], Dep()) for i in range(4)]
    sp_load(sb_view(rw, None, "p a b -> p (a b)"), rw_d)
    sp_load(rb, rb_d)
    if n_exp > 0:
        sp_load(ebg, ebg_d)
        sp_load(ebu, ebu_d)
        k.dma("pool", ebd, ebd_d)

    if mstage == 1:
        return dbg_exit()
    st_m1 = ExitStack()
    cur["st"] = st_m1
    sh2 = sb("sh2", [P, D], F32)
    A2 = sb("A2", [P, D], F32)
    h2f = sb("h2f", [P, D], F32)
    h2Tf = V(m_all[:].rearrange("p a (b c) -> p (a b) c", c=P), Dep())
    rt = sb("rt", [P, 8 * NE], F32)
    build_condrep("_m1")
    sp_load(h2f, n2g_d)
    stream(modrow_seg(3, sh2))
    stream(modrow_seg(4, A2))
    k.op("dve", lambda: nc.vector.scalar_tensor_tensor(out=A2.ap, in0=A2.ap, scalar=1.0, in1=h2f.ap, op0=ALU.add, op1=ALU.mult),
         outs=[A2], ins=[A2, h2f])
    stream(modrow_seg(5, gt2))
    if mstage == 2:
        return dbg_exit()
    for sub in range(TOK // P):
        tcols = slice(sub * P, (sub + 1) * P)
        sp_load(acc[:, sub, :], x1_d[sub * P:(sub + 1) * P, :])
        if mstage == 31:
            continue
        ssq = small[:, 8:9]
        rs = small[:, 9:10]
        k.op("act", lambda: nc.scalar.activation(out=h2f.ap, in_=acc.ap[:, sub, :], func=ACT.Square, accum_out=ssq.ap),
             outs=[h2f, ssq], ins=[acc])
        rstd_from_ssq(rs, ssq, D)
        if mstage == 32:
            continue
        k.op("dve", lambda: nc.vector.scalar_tensor_tensor(out=h2f.ap, in0=acc.ap[:, sub, :], scalar=rs.ap, in1=A2.ap,
                                                          op0=ALU.mult, op1=ALU.mult), outs=[h2f], ins=[acc, rs, A2])
        k.op("dve", lambda: nc.vector.tensor_tensor(out=h2f.ap, in0=h2f.ap, in1=sh2.ap, op=ALU.add), outs=[h2f], ins=[h2f, sh2])
        if mstage == 33:
            continue
        for kq in range(4):
            ps = psum()
            k.pe([(lambda kk=kk: nc.tensor.transpose(out=ps.ap[:, kk * P:(kk + 1) * P],
                                                    in_=h2f.ap[:, (kq * 4 + kk) * P:(kq * 4 + kk + 1) * P],
                                                    identity=ident_f.ap)) for kk in range(4)], outs=[ps], ins=[h2f, ident_f])
            k.op("act", lambda kq=kq: nc.scalar.copy(out=h2Tf.ap[:, kq * 4:(kq + 1) * 4, :],
                                                    in_=ps.ap.rearrange("p (a b) -> p a b", a=4)), outs=[h2Tf], ins=[ps])
            if mstage == 34:
                continue
            k.op("act", lambda kq=kq: nc.scalar.copy(out=h2T.ap[:, kq * 4:(kq + 1) * 4, tcols],
                                                    in_=ps.ap.rearrange("p (a b) -> p a b", a=4)), outs=[h2T], ins=[ps])
        if mstage in (3, 31, 32, 33, 34):
            continue
        psr = psum()
        k.mm(psr[:, 0:NE], [(h2Tf[:, kk, :], rw[:, kk, :]) for kk in range(KC)])
        lg = rt[:, 0:NE]
        wk = rt[:, NE:2 * NE]
        sel = rt[:, 2 * NE:3 * NE]
        eq = rt[:, 3 * NE:4 * NE]
        ex = rt[:, 4 * NE:5 * NE]
        mx = small[:, 10:11]
        m1 = small[:, 11:12]
        sm = small[:, 12:13]
        k.op("dve", lambda: nc.vector.tensor_tensor(out=lg.ap, in0=psr.ap[:, 0:NE], in1=rb.ap, op=ALU.add), outs=[lg], ins=[psr, rb])
        k.op("dve", lambda: nc.vector.tensor_copy(out=wk.ap, in_=lg.ap), outs=[wk], ins=[lg])
        k.op("dve", lambda: nc.vector.memset(sel.ap, 0.0), outs=[sel])
        for r_ in range(4):
            dst = m1 if r_ == 0 else mx
            k.op("dve", lambda dst=dst: nc.vector.reduce_max(out=dst.ap, in_=wk.ap, axis=mybir.AxisListType.X), outs=[dst], ins=[wk])
            k.op("dve", lambda dst=dst: nc.vector.tensor_scalar(out=eq.ap, in0=wk.ap, scalar1=dst.ap, scalar2=None, op0=ALU.is_equal),
                 outs=[eq], ins=[wk, dst])
            k.op("dve", lambda: nc.vector.tensor_tensor(out=sel.ap, in0=sel.ap, in1=eq.ap, op=ALU.add), outs=[sel], ins=[sel, eq])
            k.op("dve", lambda: nc.vector.scalar_tensor_tensor(out=wk.ap, in0=eq.ap, scalar=-1e30, in1=wk.ap, op0=ALU.mult, op1=ALU.add),
                 outs=[wk], ins=[eq, wk])
        k.op("dve", lambda: nc.vector.tensor_scalar(out=m1.ap, in0=m1.ap, scalar1=-1.0, scalar2=None, op0=ALU.mult), outs=[m1], ins=[m1])
        k.op("act", lambda: nc.scalar.activation(out=ex.ap, in_=lg.ap, func=ACT.Exp, bias=m1.ap), outs=[ex], ins=[lg, m1])
        k.op("dve", lambda: nc.vector.tensor_tensor(out=ex.ap, in0=ex.ap, in1=sel.ap, op=ALU.mult), outs=[ex], ins=[ex, sel])
        k.op("dve", lambda: nc.vector.reduce_sum(out=sm.ap, in_=ex.ap, axis=mybir.AxisListType.X), outs=[sm], ins=[ex])
        k.op("dve", lambda: nc.vector.reciprocal(out=sm.ap, in_=sm.ap), outs=[sm], ins=[sm])
        k.op("dve", lambda: nc.vector.tensor_scalar(out=gates.ap[:, sub, :], in0=ex.ap, scalar1=sm.ap, scalar2=None, op0=ALU.mult),
             outs=[gates], ins=[ex, sm])
        if mstage == 4:
            continue
        pst = psum()
        k.pe([lambda: nc.tensor.transpose(out=pst.ap[0:NE, 0:P], in_=gates.ap[:, sub, :], identity=ident_f.ap)],
             outs=[pst], ins=[gates, ident_f])
        k.op("act", lambda: nc.scalar.copy(out=GT.ap[:, tcols], in_=pst.ap[0:NE, 0:P]), outs=[GT], ins=[pst])
    if mstage in (3, 4, 5, 31, 32, 33, 34):
        return dbg_exit()
    k.barrier()
    st_m1.close()
    cur["st"] = st
    HT = sb("HT", [P, KC, TOK], BF16)

    wstate["n"] = (wstate["n"] + NWB - 1) // NWB * NWB
    for e in range(n_exp):
        items = []
        for bi in range(8):
            holder = {}

            def fg_(buf, holder=holder):
                holder["g"] = buf

            def fu_(buf, bi=bi, holder=holder, e=e):
                gb = holder["g"]
                for jj in range(2):
                    j = bi * 2 + jj
                    bcol = e * KC + j
                    for tt in range(TOK // 512):
                        tc = slice(tt * 512, (tt + 1) * 512)
                        psg = psum()
                        k.mm(psg, [(gb[:, kk, jj * P:(jj + 1) * P], h2T[:, kk, tc]) for kk in range(KC)])
                        psu = psum()
                        k.mm(psu, [(buf[:, kk, jj * P:(jj + 1) * P], h2T[:, kk, tc]) for kk in range(KC)])
                        k.op("dve", lambda: nc.vector.tensor_scalar(out=mA.ap, in0=psg.ap, scalar1=ebg.ap[:, bcol:bcol + 1], scalar2=7.0,
                                                                   op0=ALU.add, op1=ALU.min), outs=[mA], ins=[psg, ebg])
                        k.op("act", lambda: nc.scalar.activation(out=mB.ap, in_=mA.ap, func=ACT.Sigmoid, scale=1.702), outs=[mB], ins=[mA])
                        k.op("dve", lambda: nc.vector.tensor_scalar(out=mC.ap, in0=psu.ap, scalar1=ebu.ap[:, bcol:bcol + 1], scalar2=7.0,
                                                                   op0=ALU.add, op1=ALU.min), outs=[mC], ins=[psu, ebu])
                        k.op("dve", lambda: nc.vector.tensor_scalar(out=mC.ap, in0=mC.ap, scalar1=-7.0, scalar2=1.0,
                                                                    op0=ALU.max, op1=ALU.add), outs=[mC], ins=[mC])
                        k.op("dve", lambda: nc.vector.tensor_tensor(out=mA.ap, in0=mA.ap, in1=mB.ap, op=ALU.mult), outs=[mA], ins=[mA, mB])
                        k.op("dve", lambda: nc.vector.tensor_tensor(out=HT.ap[:, j, tc], in0=mC.ap, in1=mA.ap, op=ALU.mult),
                             outs=[HT], ins=[mC, mA])
            items.append((ewg[e * 8 + bi], fg_))
            items.append((ewu[e * 8 + bi], fu_))
        for hb in range(4):
            holder = {}

            def fd0_(buf, holder=holder):
                holder["a"] = buf
                holder["slot"] = (wstate["n"] - 1)

            def fd1_(buf, hb=hb, e=e, holder=holder):
                ba = holder["a"]
                ia = [i for i in range(NWB) if wpool[i] is ba][0]
                ib = [i for i in range(NWB) if wpool[i] is buf][0]
                assert ib == ia + 1 and ia % 2 == 0, (ia, ib)
                cols = slice(hb * 512, (hb + 1) * 512)
                for sub in range(TOK // P):
                    psy = psum()
                    k.mm(psy, [(HT[:, kk, sub * P:(sub + 1) * P], V(wp_all[:][:, ia:ia + 2, kk, :], None)) for kk in range(KC)],
                         extra_ins=[ba, buf])
                    k.op("dve", lambda: nc.vector.scalar_tensor_tensor(out=mD.ap, in0=psy.ap,
                                                                      scalar=gates.ap[:, sub, e:e + 1], in1=gt2.ap[:, cols],
                                                                      op0=ALU.mult, op1=ALU.mult), outs=[mD], ins=[psy, gates, gt2])
                    k.op("dve", lambda: nc.vector.tensor_tensor(out=acc.ap[:, sub, cols], in0=acc.ap[:, sub, cols], in1=mD.ap,
                                                               op=ALU.add), outs=[acc], ins=[acc, mD])
            items.append((ewd[e * 8 + hb * 2], fd0_))
            items.append((ewd[e * 8 + hb * 2 + 1], fd1_))
        nxt_e = []
        if e + 1 < n_exp:
            for b in range(3):
                nxt_e += [ewg[(e + 1) * 8 + b], ewu[(e + 1) * 8 + b]]
        stream(items, nxt=nxt_e)
    if n_exp > 0:
        for bi in range(8):
            cols = slice(bi * WB, (bi + 1) * WB)
            for sub in range(TOK // P):
                psy = psum()
                k.mm(psy[:, 0:WB], [(GT[0:NE, sub * P:(sub + 1) * P], ebd[0:NE, cols])])
                k.op("dve", lambda: nc.vector.tensor_tensor(out=mD.ap[:, 0:WB], in0=psy.ap[:, 0:WB], in1=gt2.ap[:, cols], op=ALU.mult),
                     outs=[mD], ins=[psy, gt2])
                k.op("dve", lambda: nc.vector.tensor_tensor(out=acc.ap[:, sub, cols], in0=acc.ap[:, sub, cols], in1=mD.ap[:, 0:WB],
                                                            op=ALU.add), outs=[acc], ins=[acc, mD])
    fgb = gt2
    sp_load(fgb, fg_d)
    junk = sb_view(HT, F32, "p a b -> p (a b)")[:, 0:D]
    last = None
    for sub in range(TOK // P):
        ssq = small[:, 14:15]
        rs = small[:, 15:16]
        k.op("act", lambda: nc.scalar.activation(out=junk.ap, in_=acc.ap[:, sub, :], func=ACT.Square, accum_out=ssq.ap),
             outs=[junk, ssq], ins=[acc])
        rstd_from_ssq(rs, ssq, D)
        k.op("dve", lambda: nc.vector.scalar_tensor_tensor(out=acc.ap[:, sub, :], in0=acc.ap[:, sub, :], scalar=rs.ap, in1=fgb.ap,
                                                          op0=ALU.mult, op1=ALU.mult), outs=[acc], ins=[acc, rs, fgb])
        last = k.dma("sp", out_d[sub * P:(sub + 1) * P, :], acc[:, sub, :])
    k.wait_tok("sp", last)
    return nc, st


def _blk(w, ncols=WB):
    n = w.shape[1] // ncols
    return np.ascontiguousarray(w.reshape(KC, P, n, ncols).transpose(2, 1, 0, 3).reshape(n, P, KC * ncols))


def _bc(v, n=P):
    return np.ascontiguousarray(np.broadcast_to(np.asarray(v, np.float32).reshape(1, -1), (n, v.size)))


def _col(v):
    return np.ascontiguousarray(v.reshape(-1, P).T)


def _shared_inputs(inp, n_exp):
    f = np.float32
    w_in = np.asarray(inp["w_in"][0], f)
    ada_w = np.asarray(inp["ada_w"][0], f)
    ada_b = np.asarray(inp["ada_b"][0], f)
    w2aug = np.zeros((32, 1024), f)
    w2aug[0:16] = inp["gla_gate_w2"][0]
    w2aug[16] = inp["gla_gate_b"][0]
    s_idx = np.arange(P)[:, None]
    t_idx = np.arange(P)[None, :]
    sh = {
        "adaw": _blk(ada_w),
        "adabc": _col(ada_b[:4096]),
        "adabr": ada_b[4096:].reshape(1, 8192).copy(),
        "g1": _col(np.asarray(inp["norm1_g"][0], f)),
        "win": _blk(np.concatenate([w_in[:, :10240], w_in[:, 10256:]], axis=1)),
        "wglr": np.ascontiguousarray(w_in[:, 10240:10256].reshape(KC, P, 16).transpose(1, 0, 2).reshape(P, KC * 16)),
        "w2aug": w2aug,
        "lng": _bc(inp["sgu_ln_g"][0]),
        "lnb": _bc(inp["sgu_ln_b"][0]),
        "wmT": np.ascontiguousarray(np.asarray(inp["sgu_w"][0], f).transpose(2, 0, 1).reshape(P, 8 * P)),
        "sgub": np.asarray(inp["sgu_b"][0], f).reshape(1, 8 * P).copy(),
        "gn": _bc(inp["gla_norm_g"][0]),
        "wa": _blk(np.asarray(inp["w_branch_a"][0], f)),
        "wb": _blk(np.asarray(inp["w_branch_b"][0], f)),
        "wo": _blk(np.asarray(inp["w_out"][0], f)),
        "n2g": _bc(inp["norm2_g"][0]),
        "rw": np.ascontiguousarray(np.asarray(inp["router_w"][0], f).reshape(KC, P, NE).transpose(1, 0, 2).reshape(P, KC * NE)),
        "rb": _bc(inp["router_b"][0]),
        "fg": _bc(inp["final_g"]),
        "ident": np.eye(P, dtype=f),
        "tri": np.where(s_idx <= t_idx, -1.0 / 16.0, 0.0).astype(f),
        "trirev": np.where(s_idx > t_idx, -1.0 / 16.0, 0.0).astype(f),
        "cmask": np.where(s_idx <= t_idx, 1.0, 0.0).astype(f),
    }
    if n_exp > 0:
        sh["ewg"] = np.concatenate([_blk(np.asarray(inp["exp_w_gate"][0, e], f)) for e in range(n_exp)], axis=0)
        sh["ewu"] = np.concatenate([_blk(np.asarray(inp["exp_w_up"][0, e], f)) for e in range(n_exp)], axis=0)
        sh["ewd"] = np.concatenate([_blk(np.asarray(inp["exp_w_down"][0, e], f)) for e in range(n_exp)], axis=0)
        sh["ebg"] = np.ascontiguousarray(np.asarray(inp["exp_b_gate"][0], f).reshape(NE, KC, P).transpose(2, 0, 1).reshape(P, NE * KC))
        sh["ebu"] = np.ascontiguousarray(np.asarray(inp["exp_b_up"][0], f).reshape(NE, KC, P).transpose(2, 0, 1).reshape(P, NE * KC))
        sh["ebd"] = np.ascontiguousarray(np.asarray(inp["exp_b_down"][0], f))
    return sh


def _in_maps(inp, n_exp=NE):
    sh = _shared_inputs(inp, n_exp)
    x = np.asarray(inp["x"], np.float32)
    c = np.asarray(inp["c"], np.float32)
    maps = []
    for core in range(8):
        b, half = core // 2, core % 2
        m = dict(sh)
        m["xo"] = np.ascontiguousarray(x[b, half * TOK:(half + 1) * TOK])
        m["xp"] = np.ascontiguousarray(x[b, 0:TOK]) if half == 1 else np.zeros((TOK, D), np.float32)
        m["flag"] = np.full((P, 1), float(half), np.float32)
        m["cT"] = _col(c[b])
        maps.append(m)
    return maps


def kernel(**inputs):
    nc, st = _build(NE, None)
    maps = _in_maps(inputs, NE)
    res = run_bass_kernel_spmd(nc, maps, core_ids=list(range(8)))
    out = np.zeros((4, 2 * TOK, D), np.float32)
    for core in range(8):
        b, half = core // 2, core % 2
        out[b, half * TOK:(half + 1) * TOK] = np.asarray(res.results[core]["out"], np.float32)
    return out
```
